# Optimizing a Trainium2 kernel written in Bass

```python
import math
import jax, jax.numpy as jnp
from jax import lax
import numpy as np

D_MODEL = 1024
BATCH = 8
SEQ = 4096
DEPTH = 2

HEAD_DIM = 64
N_HEAD_SLOTS = D_MODEL // HEAD_DIM
MIX_WIDTH = N_HEAD_SLOTS * HEAD_DIM
HEADS_PER_MIXER = N_HEAD_SLOTS // 2
KV_HEADS = 2
GROUP = HEADS_PER_MIXER // KV_HEADS
HQ = HEADS_PER_MIXER * HEAD_DIM
KVW = KV_HEADS * HEAD_DIM
Q_BLOCK = 128
NSA_CMP_LEN = 32
NSA_CMP_STRIDE = 16
NSA_SEL_LEN = 64
NSA_SEL_TOP = 8
NSA_WINDOW = 512
NSA_FORCE = 1e4
SWA_WINDOW = 128
MOBA_BLOCK = 256
MOBA_TOP = 3
MOBA_Q_CHUNK = 32
REL_BUCKETS = 32
REL_MAX_DIST = 128
D_FF = ((8 * D_MODEL // 3 + 255) // 256) * 256
RMS_EPS = 1e-6
NEG = -1e30
AB_WIDTH = HQ + 6 * KVW + 3 * HEADS_PER_MIXER + HQ + 2 * KVW
CD_WIDTH = HQ + 2 * KVW + 3 * HQ
N_EVEN = (DEPTH + 1) // 2
N_ODD = DEPTH // 2

kernel_name = "hybrid_nsa_swa_moba_stickbreak_block"


def rms_norm(x, w):
    xf = x.astype(jnp.float32)
    y = xf * lax.rsqrt(jnp.mean(xf * xf, axis=-1, keepdims=True) + RMS_EPS)
    return (y * w.astype(jnp.float32)).astype(x.dtype)


def rel_bucket(dist):
    n = jnp.maximum(dist, 0)
    max_exact = REL_BUCKETS // 2
    nf = jnp.maximum(n, 1).astype(jnp.float32)
    large = max_exact + (jnp.log(nf / max_exact) / math.log(REL_MAX_DIST / max_exact)
                         * (REL_BUCKETS - max_exact)).astype(jnp.int32)
    large = jnp.minimum(large, REL_BUCKETS - 1)
    return jnp.where(n < max_exact, n, large)


def masked_softmax(s, valid):
    s = jnp.where(valid, s.astype(jnp.float32), NEG)
    p = jax.nn.softmax(s, axis=-1)
    return jnp.where(valid, p, 0.0)


def nsa_attention(q, k_cmp, v_cmp, k_sel, v_sel, k_win, v_win, gates, cmp_wk, cmp_wv, cmp_pe, rel_tab):
    B, S, H, dh = q.shape
    G, R = KV_HEADS, GROUP
    dt = q.dtype
    scale = HEAD_DIM ** -0.5
    n_cmp = (S - NSA_CMP_LEN) // NSA_CMP_STRIDE + 1
    cmp_start = jnp.arange(n_cmp) * NSA_CMP_STRIDE
    gidx = cmp_start[:, None] + jnp.arange(NSA_CMP_LEN)[None, :]
    kc = jnp.einsum('bnlgd,lde->bnge', k_cmp[:, gidx] + cmp_pe[:, None, :], cmp_wk)
    vc = jnp.einsum('bnlgd,lde->bnge', v_cmp[:, gidx] + cmp_pe[:, None, :], cmp_wv)
    cmp_end = cmp_start + NSA_CMP_LEN - 1
    n_sel = S // NSA_SEL_LEN
    sel_start = jnp.arange(n_sel) * NSA_SEL_LEN
    overlap = jnp.maximum(jnp.minimum(cmp_end[:, None] + 1, sel_start[None, :] + NSA_SEL_LEN)
                          - jnp.maximum(cmp_start[:, None], sel_start[None, :]), 0)
    overlap_w = overlap.astype(jnp.float32) / NSA_CMP_LEN
    top = min(NSA_SEL_TOP, n_sel)
    ksb = k_sel.reshape(B, n_sel, NSA_SEL_LEN, G, dh).transpose(0, 3, 1, 2, 4)
    vsb = v_sel.reshape(B, n_sel, NSA_SEL_LEN, G, dh).transpose(0, 3, 1, 2, 4)
    kw = jnp.pad(k_win, ((0, 0), (NSA_WINDOW, 0), (0, 0), (0, 0)))
    vw = jnp.pad(v_win, ((0, 0), (NSA_WINDOW, 0), (0, 0), (0, 0)))
    bi = jnp.arange(B)[:, None, None, None]
    gi = jnp.arange(G)[None, :, None, None]
    sb = jnp.arange(n_sel)

    def block(n):
        t0 = n * Q_BLOCK
        tq = t0 + jnp.arange(Q_BLOCK)
        qb = lax.dynamic_slice_in_dim(q, t0, Q_BLOCK, axis=1).reshape(B, Q_BLOCK, G, R, dh)
        gb = lax.dynamic_slice_in_dim(gates, t0, Q_BLOCK, axis=1).reshape(B, Q_BLOCK, G, R, 1, 3)
        dist = tq[:, None] - cmp_end[None, :]
        s = jnp.einsum('bqgrd,bngd->bgrqn', qb, kc) * scale
        s = s + rel_tab[rel_bucket(dist)].transpose(2, 3, 0, 1)
        p_cmp = masked_softmax(s, dist >= 0)
        o_cmp = jnp.einsum('bgrqn,bngd->bqgrd', p_cmp.astype(dt), vc)
        imp = jnp.einsum('bgrqn,ns->bgqs', p_cmp, overlap_w)
        own = tq // NSA_SEL_LEN
        forced = (sb[None, :] == 0) | (sb[None, :] == own[:, None]) | (sb[None, :] == own[:, None] - 1)
        causal_blk = sb[None, :] <= own[:, None]
        imp = jnp.where(causal_blk, imp + jnp.where(forced, NSA_FORCE, 0.0), NEG)
        _, sel = lax.top_k(imp, top)
        ks = ksb[bi, gi, sel].reshape(B, G, Q_BLOCK, top * NSA_SEL_LEN, dh)
        vs = vsb[bi, gi, sel].reshape(B, G, Q_BLOCK, top * NSA_SEL_LEN, dh)
        pos = (sel[..., None] * NSA_SEL_LEN + jnp.arange(NSA_SEL_LEN)).reshape(B, G, Q_BLOCK, top * NSA_SEL_LEN)
        dist = tq[None, None, :, None] - pos
        s = jnp.einsum('bqgrd,bgqkd->bgrqk', qb, ks) * scale
        s = s + rel_tab[rel_bucket(dist), gi].transpose(0, 1, 4, 2, 3)
        p = masked_softmax(s, (dist >= 0)[:, :, None])
        o_sel = jnp.einsum('bgrqk,bgqkd->bqgrd', p.astype(dt), vs)
        kwb = lax.dynamic_slice_in_dim(kw, t0, NSA_WINDOW + Q_BLOCK, axis=1)
        vwb = lax.dynamic_slice_in_dim(vw, t0, NSA_WINDOW + Q_BLOCK, axis=1)
        kpos = t0 - NSA_WINDOW + jnp.arange(NSA_WINDOW + Q_BLOCK)
        dist = tq[:, None] - kpos[None, :]
        valid = (dist >= 0) & (dist < NSA_WINDOW) & (kpos[None, :] >= 0)
        s = jnp.einsum('bqgrd,bkgd->bgrqk', qb, kwb) * scale
        s = s + rel_tab[rel_bucket(dist)].transpose(2, 3, 0, 1)
        p = masked_softmax(s, valid)
        o_win = jnp.einsum('bgrqk,bkgd->bqgrd', p.astype(dt), vwb)
        o = jnp.stack([o_cmp, o_sel, o_win], axis=-1)
        return jnp.sum(o * gb, axis=-1).reshape(B, Q_BLOCK, H * dh)

    out = lax.map(block, jnp.arange(S // Q_BLOCK))
    return out.transpose(1, 0, 2, 3).reshape(B, S, H * dh)


def swa_sink_attention(q, k, v, sinks, rel_tab):
    B, S, H, dh = q.shape
    G, R = KV_HEADS, GROUP
    dt = q.dtype
    scale = HEAD_DIM ** -0.5
    kp = jnp.pad(k, ((0, 0), (SWA_WINDOW, 0), (0, 0), (0, 0)))
    vp = jnp.pad(v, ((0, 0), (SWA_WINDOW, 0), (0, 0), (0, 0)))
    sink = jnp.broadcast_to(sinks.astype(jnp.float32).reshape(G, R)[None, :, :, None, None],
                            (B, G, R, Q_BLOCK, 1))

    def block(n):
        t0 = n * Q_BLOCK
        tq = t0 + jnp.arange(Q_BLOCK)
        qb = lax.dynamic_slice_in_dim(q, t0, Q_BLOCK, axis=1).reshape(B, Q_BLOCK, G, R, dh)
        kb = lax.dynamic_slice_in_dim(kp, t0, SWA_WINDOW + Q_BLOCK, axis=1)
        vb = lax.dynamic_slice_in_dim(vp, t0, SWA_WINDOW + Q_BLOCK, axis=1)
        kpos = t0 - SWA_WINDOW + jnp.arange(SWA_WINDOW + Q_BLOCK)
        dist = tq[:, None] - kpos[None, :]
        valid = (dist >= 0) & (dist < SWA_WINDOW) & (kpos[None, :] >= 0)
        s = jnp.einsum('bqgrd,bkgd->bgrqk', qb, kb) * scale
        s = s + rel_tab[rel_bucket(dist)].transpose(2, 3, 0, 1)
        s = jnp.where(valid, s.astype(jnp.float32), NEG)
        p = jax.nn.softmax(jnp.concatenate([s, sink], axis=-1), axis=-1)[..., :-1]
        o = jnp.einsum('bgrqk,bkgd->bqgrd', p.astype(dt), vb)
        return o.reshape(B, Q_BLOCK, H * dh)

    out = lax.map(block, jnp.arange(S // Q_BLOCK))
    return out.transpose(1, 0, 2, 3).reshape(B, S, H * dh)


def moba_attention(q, k, v, rel_tab_h):
    B, S, H, dh = q.shape
    G, R = KV_HEADS, GROUP
    L = MOBA_BLOCK
    dt = q.dtype
    scale = HEAD_DIM ** -0.5
    nb = max(-(-S // L), 2)
    pad = nb * L - S
    kp = jnp.pad(k, ((0, 0), (0, pad), (0, 0), (0, 0)))
    vp = jnp.pad(v, ((0, 0), (0, pad), (0, 0), (0, 0)))
    head_grp = jnp.arange(H) // R
    kb = kp.reshape(B, nb, L, G, dh)
    k_mean = jnp.mean(kb.astype(jnp.float32), axis=2).astype(dt)[:, :, head_grp]
    kbh = kb[:, :, :, head_grp].transpose(0, 3, 1, 2, 4)
    vbh = vp.reshape(B, nb, L, G, dh)[:, :, :, head_grp].transpose(0, 3, 1, 2, 4)
    top = min(MOBA_TOP, nb - 1)
    bi = jnp.arange(B)[:, None, None, None]
    hi = jnp.arange(H)[None, None, :, None]
    Qc = MOBA_Q_CHUNK

    def chunk(n):
        t0 = n * Qc
        tq = t0 + jnp.arange(Qc)
        qc = lax.dynamic_slice_in_dim(q, t0, Qc, axis=1)
        own = t0 // L
        gs = jnp.einsum('bqhd,bnhd->bqhn', qc, k_mean).astype(jnp.float32)
        gs = jnp.where(jnp.arange(nb) < own, gs, NEG)
        _, sel = lax.top_k(gs, top)
        sel_ok = sel < own
        ks = kbh[bi, hi, sel]
        vs = vbh[bi, hi, sel]
        pos = sel[..., None] * L + jnp.arange(L)
        s_past = jnp.einsum('bqhd,bqhnld->bqhnl', qc, ks) * scale
        s_past = s_past + rel_tab_h[rel_bucket(tq[None, :, None, None, None] - pos), hi[..., None]]
        k_own = lax.dynamic_slice_in_dim(kp, own * L, L, axis=1)[:, :, head_grp]
        v_own = lax.dynamic_slice_in_dim(vp, own * L, L, axis=1)[:, :, head_grp]
        dist_own = tq[:, None] - (own * L + jnp.arange(L))[None, :]
        s_own = jnp.einsum('bqhd,blhd->bqhl', qc, k_own) * scale
        s_own = s_own + rel_tab_h[rel_bucket(dist_own)].transpose(0, 2, 1)
        s = jnp.concatenate([s_past.reshape(B, Qc, H, top * L), s_own], axis=-1)
        valid = jnp.concatenate([
            jnp.broadcast_to(sel_ok[..., None], (B, Qc, H, top, L)).reshape(B, Qc, H, top * L),
            jnp.broadcast_to((dist_own >= 0)[:, None, :], (B, Qc, H, L))], axis=-1)
        p = masked_softmax(s, valid).astype(dt)
        o = (jnp.einsum('bqhk,bqhkd->bqhd', p[..., :top * L], vs.reshape(B, Qc, H, top * L, dh))
             + jnp.einsum('bqhl,blhd->bqhd', p[..., top * L:], v_own))
        return o.reshape(B, Qc, H * dh)

    out = lax.map(chunk, jnp.arange(S // Qc))
    return out.transpose(1, 0, 2, 3).reshape(B, S, H * dh)


def stick_breaking_attention(q, k, v):
    B, S, H, dh = q.shape
    dt = q.dtype
    scale = HEAD_DIM ** -0.5
    kpos = jnp.arange(S)

    def block(n):
        t0 = n * Q_BLOCK
        tq = t0 + jnp.arange(Q_BLOCK)
        qb = lax.dynamic_slice_in_dim(q, t0, Q_BLOCK, axis=1)
        z = jnp.einsum('bqhd,bkhd->bhqk', qb, k).astype(jnp.float32) * scale
        strict = kpos[None, :] < tq[:, None]
        log_1m = jnp.where(strict, jax.nn.log_sigmoid(-z), 0.0)
        after = lax.cumsum(log_1m, axis=3, reverse=True) - log_1m
        w = jnp.where(strict, jnp.exp(jax.nn.log_sigmoid(z) + after), 0.0)
        o = jnp.einsum('bhqk,bkhd->bqhd', w.astype(dt), v)
        return o.reshape(B, Q_BLOCK, H * dh)

    out = lax.map(block, jnp.arange(S // Q_BLOCK))
    return out.transpose(1, 0, 2, 3).reshape(B, S, H * dh)


def mixer_ab(h, w_in, w_out, cmp_wk, cmp_wv, cmp_pe, sinks, rel_table):
    B, S, _ = h.shape
    H, G, dh = HEADS_PER_MIXER, KV_HEADS, HEAD_DIM
    proj = h @ w_in
    sizes = [HQ, KVW, KVW, KVW, KVW, KVW, KVW, 3 * H, HQ, KVW, KVW]
    qa, kca, vca, ksa, vsa, kwa, vwa, ga, qb, kb, vb = jnp.split(proj, np.cumsum(sizes)[:-1].tolist(), axis=-1)
    kv = lambda t: t.reshape(B, S, G, dh)
    gates = jax.nn.sigmoid(ga.astype(jnp.float32)).astype(h.dtype).reshape(B, S, H, 3)
    tab_a = rel_table[:, :H].reshape(REL_BUCKETS, G, GROUP)
    tab_b = rel_table[:, H:2 * H].reshape(REL_BUCKETS, G, GROUP)
    o_a = nsa_attention(qa.reshape(B, S, H, dh), kv(kca), kv(vca), kv(ksa), kv(vsa), kv(kwa), kv(vwa),
                        gates, cmp_wk, cmp_wv, cmp_pe, tab_a)
    o_b = swa_sink_attention(qb.reshape(B, S, H, dh), kv(kb), kv(vb), sinks, tab_b)
    return jnp.concatenate([o_a, o_b], axis=-1) @ w_out


def mixer_cd(h, w_in, w_out, rel_table):
    B, S, _ = h.shape
    H, G, dh = HEADS_PER_MIXER, KV_HEADS, HEAD_DIM
    proj = h @ w_in
    sizes = [HQ, KVW, KVW, HQ, HQ, HQ]
    qc, kc, vc, qd, kd, vd = jnp.split(proj, np.cumsum(sizes)[:-1].tolist(), axis=-1)
    o_c = moba_attention(qc.reshape(B, S, H, dh), kc.reshape(B, S, G, dh), vc.reshape(B, S, G, dh),
                         rel_table[:, :H])
    o_d = stick_breaking_attention(qd.reshape(B, S, H, dh), kd.reshape(B, S, H, dh), vd.reshape(B, S, H, dh))
    return jnp.concatenate([o_c, o_d], axis=-1) @ w_out


def swiglu(h, w_in, w_out):
    gate, up = jnp.split(h @ w_in, 2, axis=-1)
    return (jax.nn.silu(gate) * up) @ w_out


def setup_inputs(seed: int = 0) -> dict:
    key = jax.random.key(seed)
    ks = jax.random.split(key, 16)
    f32 = jnp.float32

    def nrm(k, shape, s):
        return jax.random.normal(k, shape, f32) * s

    return {
        "x": nrm(ks[0], (BATCH, SEQ, D_MODEL), 1.0),
        "c": nrm(ks[1], (BATCH, D_MODEL), 1.0),
        "rel_table": nrm(ks[2], (REL_BUCKETS, N_HEAD_SLOTS), 0.5),
        "mod_w": nrm(ks[3], (DEPTH, 2, D_MODEL, 3 * D_MODEL), 0.5 * D_MODEL ** -0.5),
        "mod_b": nrm(ks[4], (DEPTH, 2, 3 * D_MODEL), 0.02),
        "norm_w": 1.0 + nrm(ks[5], (DEPTH, 2, 2, D_MODEL), 0.02),
        "w_in_ab": nrm(ks[6], (N_EVEN, D_MODEL, AB_WIDTH), D_MODEL ** -0.5),
        "w_out_ab": nrm(ks[7], (N_EVEN, MIX_WIDTH, D_MODEL), MIX_WIDTH ** -0.5),
        "nsa_cmp_wk": nrm(ks[8], (N_EVEN, NSA_CMP_LEN, HEAD_DIM, HEAD_DIM), (NSA_CMP_LEN * HEAD_DIM) ** -0.5),
        "nsa_cmp_wv": nrm(ks[9], (N_EVEN, NSA_CMP_LEN, HEAD_DIM, HEAD_DIM), (NSA_CMP_LEN * HEAD_DIM) ** -0.5),
        "nsa_cmp_pe": nrm(ks[10], (N_EVEN, NSA_CMP_LEN, HEAD_DIM), 0.1),
        "swa_sinks": nrm(ks[11], (N_EVEN, HEADS_PER_MIXER), 0.5),
        "w_in_cd": nrm(ks[12], (N_ODD, D_MODEL, CD_WIDTH), D_MODEL ** -0.5),
        "w_out_cd": nrm(ks[13], (N_ODD, MIX_WIDTH, D_MODEL), MIX_WIDTH ** -0.5),
        "ffn_w_in": nrm(ks[14], (DEPTH, D_MODEL, 2 * D_FF), D_MODEL ** -0.5),
        "ffn_w_out": nrm(ks[15], (DEPTH, D_FF, D_MODEL), D_FF ** -0.5),
    }


def reference(x, c, rel_table, mod_w, mod_b, norm_w, w_in_ab, w_out_ab, nsa_cmp_wk, nsa_cmp_wv,
              nsa_cmp_pe, swa_sinks, w_in_cd, w_out_cd, ffn_w_in, ffn_w_out):
    for layer in range(DEPTH):
        shift, scale, gate = jnp.split((c @ mod_w[layer, 0] + mod_b[layer, 0])[:, None, :], 3, axis=-1)
        h = rms_norm(x, norm_w[layer, 0, 0]) * (1.0 + scale) + shift
        i = layer // 2
        if layer % 2 == 0:
            y = mixer_ab(h, w_in_ab[i], w_out_ab[i], nsa_cmp_wk[i], nsa_cmp_wv[i], nsa_cmp_pe[i],
                         swa_sinks[i], rel_table)
        else:
            y = mixer_cd(h, w_in_cd[i], w_out_cd[i], rel_table)
        x = x + gate * rms_norm(y, norm_w[layer, 0, 1])
        shift, scale, gate = jnp.split((c @ mod_w[layer, 1] + mod_b[layer, 1])[:, None, :], 3, axis=-1)
        h = rms_norm(x, norm_w[layer, 1, 0]) * (1.0 + scale) + shift
        y = swiglu(h, ffn_w_in[layer], ffn_w_out[layer])
        x = x + gate * rms_norm(y, norm_w[layer, 1, 1])
    return x
```

```python
import math
import os
from contextlib import ExitStack

import numpy as np
import concourse.bass as bass
import concourse.mybir as mybir
from concourse.bass_utils import run_bass_kernel_spmd

F32 = mybir.dt.float32
BF16 = mybir.dt.bfloat16
AF = mybir.ActivationFunctionType
ALU = mybir.AluOpType
AX = mybir.AxisListType

S = 4096
D = 1024
NT = S // 128
DFF = 2816
NFC = DFF // 128
NEG = -30000.0
EPS = 1e-6
PERM = [0, 2, 1, 3]


class Buf:
    def __init__(self, t, name=""):
        self.t = t
        self.name = name
        self.w = {}
        self.r = {}

    def __getitem__(self, idx):
        return self.t[idx]

    def ap(self):
        return self.t.ap()


class FW:
    def __init__(self, nc, es, n_dma_slots=32):
        self.nc = nc
        self.es = es
        self.eng = {"pe": nc.tensor, "act": nc.scalar, "dve": nc.vector, "pool": nc.gpsimd, "sp": nc.sync}
        self.sem = {}
        self.cnt = {}
        for k in self.eng:
            self.sem[k] = es.enter_context(nc.semaphore("s_" + k))
            self.cnt[k] = 0
        self.nslots = n_dma_slots
        for i in range(n_dma_slots):
            k = "d%d" % i
            self.sem[k] = es.enter_context(nc.semaphore("s_" + k))
            self.cnt[k] = 0
        self.next_slot = 0
        self.seen = {e: {} for e in self.eng}
        self.n_ops = 0

    def _uniq(self, name):
        self.n_names = getattr(self, "n_names", 0) + 1
        return "%s_u%d" % (name, self.n_names)

    def sb(self, es, name, shape, dt):
        name = self._uniq(name)
        return Buf(es.enter_context(self.nc.sbuf_tensor(name, shape, dt)), name)

    def ps(self, es, name, shape, dt=F32):
        name = self._uniq(name)
        return Buf(es.enter_context(self.nc.psum_tensor(name, shape, dt)), name)

    def view(self, b, name=""):
        return Buf(b.t, name or b.name)

    def _wait(self, e, toks):
        for k, v in toks.items():
            if v <= 0 or self.seen[e].get(k, 0) >= v:
                continue
            self.eng[e].wait_ge(self.sem[k], v)
            self.seen[e][k] = v

    @staticmethod
    def _merge(dst, src):
        for k, v in src.items():
            if v > dst.get(k, 0):
                dst[k] = v

    def _deps(self, reads, writes):
        toks = {}
        for b in reads:
            self._merge(toks, b.w)
        for b in writes:
            self._merge(toks, b.w)
            self._merge(toks, b.r)
        return toks

    def _record(self, key, val, reads, writes):
        for b in writes:
            b.r = {}
            if val > b.w.get(key, 0):
                b.w[key] = val
        for b in reads:
            if val > b.r.get(key, 0):
                b.r[key] = val

    def op(self, e, fn, reads=(), writes=()):
        self._wait(e, self._deps(reads, writes))
        ins = fn()
        self.cnt[e] += 1
        ins.then_inc(self.sem[e], 1)
        self._record(e, self.cnt[e], reads, writes)
        self.n_ops += 1

    def dma(self, q, out, in_, reads=(), writes=()):
        s = "d%d" % self.next_slot
        self.next_slot = (self.next_slot + 1) % self.nslots
        toks = self._deps(reads, writes)
        self._merge(toks, {s: self.cnt[s]})
        self._wait(q, toks)
        ins = self.eng[q].dma_start(out=out, in_=in_)
        self.cnt[s] += 16
        ins.then_inc(self.sem[s], 16)
        self._record(s, self.cnt[s], reads, writes)
        self.n_ops += 1

    def barrier(self):
        toks = dict(self.cnt)
        for e in self.eng:
            self._wait(e, toks)


def _rel_bucket(dist):
    n = np.maximum(dist, 0)
    nf = np.maximum(n, 1).astype(np.float32)
    large = 16 + (np.log(nf / np.float32(16)) / np.float32(math.log(128 / 16)) * np.float32(16)).astype(np.int32)
    large = np.minimum(large, 31)
    return np.where(n < 16, n, large).astype(np.int64)


def _host_tables(rel_table):
    kk = np.arange(128)[:, None]
    qq = np.arange(128)[None, :]
    idx0 = _rel_bucket(qq - kk)
    idx1 = _rel_bucket(128 + qq - kk)
    cols = [4 * g + PERM[s] for g in range(2) for s in range(4)]
    cols16 = cols + [8 + c for c in cols]
    toe0 = rel_table[idx0][:, :, cols16].transpose(0, 2, 1)
    toe1 = rel_table[idx1][:, :, cols16].transpose(0, 2, 1)
    b31 = np.broadcast_to(rel_table[31][cols16][None, :], (128, 16))
    def tc(ms):
        m = np.asarray(ms)[:, None]
        d = qq - 16 * m - 31
        t = rel_table[_rel_bucket(d)][:, :, cols[:8]].transpose(0, 2, 1)
        mk = np.where(d >= 0, 0.0, NEG).astype(np.float32)
        full_t = np.zeros((128, 8, 128), np.float32)
        full_m = np.full((128, 128), 0.0, np.float32)
        full_t[: len(ms)] = t
        full_m[: len(ms)] = mk
        return full_t, full_m
    tcg, mcg = tc(range(-9, 7))
    tc0, mc0 = tc(range(0, 7))
    tc1, mc1 = tc(range(-8, 7))
    mask0 = np.where(qq >= kk, 0.0, NEG).astype(np.float32)
    masklt = np.where(qq < kk, 0.0, NEG).astype(np.float32)
    ident = np.eye(128, dtype=np.float32)
    tri = np.where(kk >= qq, -1.0, 0.0).astype(np.float32)
    q512 = np.arange(512)[None, :]
    sbm = np.stack([(128 * i + kk < q512).astype(np.float32) for i in range(4)], axis=1)
    n = np.arange(255)[:, None]
    s = np.arange(64)[None, :]
    ov = np.maximum(np.minimum(16 * n + 32, 64 * s + 64) - np.maximum(16 * n, 64 * s), 0).astype(np.float32) / 32.0
    ovw = np.zeros((128, 2, 64), np.float32)
    ovw[:, 0] = ov[:128]
    ovw[:127, 1] = ov[128:]
    ebig = (np.arange(64)[:, None] == (np.arange(4096)[None, :] // 64)).astype(np.float32)
    ebig128 = np.zeros((128, 4096), np.float32)
    ebig128[:64] = ebig
    tabs = np.concatenate([
        toe0.reshape(128, -1), toe1.reshape(128, -1), b31,
        tcg.reshape(128, -1), tc0.reshape(128, -1), tc1.reshape(128, -1)], axis=1).astype(np.float32)
    consts = np.concatenate([
        mask0, masklt, mcg, mc0, mc1, ident, tri, sbm.reshape(128, -1), ovw.reshape(128, -1)], axis=1).astype(np.float32)
    return np.ascontiguousarray(tabs), np.ascontiguousarray(consts), np.ascontiguousarray(ebig128)


T_TOE0 = 0
T_TOE1 = T_TOE0 + 16 * 128
T_B31 = T_TOE1 + 16 * 128
T_TCG = T_B31 + 16
T_TC0 = T_TCG + 8 * 128
T_TC1 = T_TC0 + 8 * 128
T_END = T_TC1 + 8 * 128
C_MASK0 = 0
C_MASKLT = 128
C_MCG = 256
C_MC0 = 384
C_MC1 = 512
C_IDENT = 640
C_TRI = 768
C_SBM = 896
C_OVW = C_SBM + 2048
C_END = C_OVW + 128


class Prog:
    def __init__(self, dbg=(), as_input=()):
        self.dbg = set(dbg)
        self.as_input = set(as_input)
        self.nc = bass.Bass("TRN2", target_bir_lowering=False)
        self.dram = {}

    def din(self, name, shape, dt=F32):
        t = self.nc.dram_tensor(name, list(shape), dt, kind="ExternalInput")
        self.dram[name] = Buf(t, name)
        self.in_names.append(name)
        return self.dram[name]

    def dscr(self, name, shape, dt=BF16, out=False):
        kind = "ExternalOutput" if (out or name in self.dbg) else "Internal"
        if name in self.as_input:
            kind = "ExternalInput"
        t = self.nc.dram_tensor(name, list(shape), dt, kind=kind)
        self.dram[name] = Buf(t, name)
        if kind == "ExternalInput":
            self.in_names.append(name)
        if kind == "ExternalOutput":
            self.out_names.append(name)
        return self.dram[name]

    def dump(self, name, buf, ap, shape, dt=F32):
        if name not in self.dbg:
            return
        fw, nc = self.fw, self.nc
        if not dict.__contains__(self.dram, name):
            self.dscr(name, shape, dt)
        fw.dma("pool", self.dram[name].ap(), ap, reads=[buf], writes=[self.dram[name]])

    def load_w(self, es, dst, dst_c0, src, r0, c0, ncols, nk, tag, dup=False):
        fw, nc = self.fw, self.nc
        srcv = src.ap()[r0:r0 + nk * 128, :].rearrange("(k p) c -> p k c", p=128)
        CW = 2048 // nk if nk <= 8 else 64
        CW = max(64, min(512, CW))
        for cc in range(0, ncols, CW):
            w = min(CW, ncols - cc)
            st = self.wstage[self.wstage_i % len(self.wstage)]
            self.wstage_i += 1
            stv = st[:, 0:nk * w].rearrange("p (k c) -> p k c", k=nk)
            fw.dma("sp", stv, srcv[:, :, c0 + cc:c0 + cc + w], reads=[src], writes=[st])
            e = ["pool", "dve"][self.wstage_i % 2]
            eng = fw.eng[e]
            fw.op(e, lambda: eng.tensor_copy(dst[:, 0:nk, dst_c0 + cc:dst_c0 + cc + w], stv), [st], [dst])
            if dup:
                e2 = ["dve", "pool"][self.wstage_i % 2]
                eng2 = fw.eng[e2]
                fw.op(e2, lambda: eng2.tensor_copy(dst[:, 0:nk, dst_c0 + 64 + cc:dst_c0 + 64 + cc + w], stv), [st], [dst])

    def alloc_wstage(self, es):
        self.wstage = [self.fw.sb(es, "wst%d" % i, [128, 2048], F32) for i in range(2)]
        self.wstage_i = 0

    def load_bcast(self, dst, src_buf, src_row_ap):
        self.fw.dma("sp", dst[:, :], src_row_ap.partition_broadcast(128), reads=[src_buf], writes=[dst])

    def rstd_from_ss(self, ss, tmp, rstd):
        fw, nc = self.fw, self.nc
        fw.op("act", lambda: nc.scalar.activation(tmp[:, :], ss[:, :], AF.Ln, scale=1.0 / D, bias=self.eps_t[:, 0:1]), [ss, self.eps_t], [tmp])
        fw.op("act", lambda: nc.scalar.activation(rstd[:, :], tmp[:, :], AF.Exp, scale=-0.5), [tmp], [rstd])

    def norm_mod_T(self, xt, A, Bv, hT_dst, hT_buf, col0, tmps, tp_ps, n_tok_cols=128):
        fw, nc = self.fw, self.nc
        junk, ss, s1, rstd, h32, hb = tmps
        fw.op("act", lambda: nc.scalar.activation(junk[:, :], xt[:, :], AF.Square, accum_out=ss[:, :]), [xt], [junk, ss])
        self.rstd_from_ss(ss, s1, rstd)
        fw.op("dve", lambda: nc.vector.scalar_tensor_tensor(h32[:, :], xt[:, :], rstd[:, 0:1], A[:, :], ALU.mult, ALU.mult), [xt, rstd, A], [h32])
        fw.op("pool", lambda: nc.gpsimd.tensor_tensor(hb[:, :], h32[:, :], Bv[:, :], ALU.add), [h32, Bv], [hb])
        for k in range(8):
            fw.op("pe", lambda: nc.tensor.transpose(tp_ps[:, k * 128:(k + 1) * 128], hb[:, k * 128:(k + 1) * 128], self.identb[:, :]), [hb, self.identb], [tp_ps])
        fw.op("act", lambda: nc.scalar.copy(hT_dst[:, :, col0:col0 + 128], tp_ps[:, :].rearrange("p (k c) -> p k c", k=8)), [tp_ps], [hT_buf])

    def post_norm_residual(self, yps, xt, Gv, xn, tmps):
        fw, nc = self.fw, self.nc
        junk, ss2, ss, s1, rstd, t32 = tmps
        for h in range(2):
            fw.op("act", lambda: nc.scalar.activation(junk[:, h * 512:(h + 1) * 512], yps[h][:, :], AF.Square, accum_out=ss2[:, h:h + 1]), [yps[h]], [junk, ss2])
        fw.op("dve", lambda: nc.vector.tensor_tensor(ss[:, :], ss2[:, 0:1], ss2[:, 1:2], ALU.add), [ss2], [ss])
        self.rstd_from_ss(ss, s1, rstd)
        for h in range(2):
            sl = slice(h * 512, (h + 1) * 512)
            fw.op("dve", lambda: nc.vector.scalar_tensor_tensor(t32[:, sl], yps[h][:, :], rstd[:, 0:1], Gv[:, sl], ALU.mult, ALU.mult), [yps[h], rstd, Gv], [t32])
        fw.op("pool", lambda: nc.gpsimd.tensor_tensor(xn[:, :], t32[:, :], xt[:, :], ALU.add), [t32, xt], [xn])

    def phase_setup(self, es):
        fw, nc = self.fw, self.nc
        d = self.dram
        self.eps_t = fw.sb(es, "eps_t", [128, 1], F32)
        fw.op("pool", lambda: nc.gpsimd.memset(self.eps_t[:, :], EPS), [], [self.eps_t])
        self.identb = fw.sb(es, "identb", [128, 128], BF16)
        self.identf = fw.sb(es, "identf", [128, 128], F32)
        fw.dma("sp", self.identf[:, :], d["consts"][:, C_IDENT:C_IDENT + 128], reads=[d["consts"]], writes=[self.identf])
        fw.op("dve", lambda: nc.vector.tensor_copy(self.identb[:, :], self.identf[:, :]), [self.identf], [self.identb])

    def phase_modvec(self):
        fw, nc = self.fw, self.nc
        d = self.dram
        with ExitStack() as es:
            cT = fw.sb(es, "cT", [128, 8], F32)
            fw.dma("sp", cT[:, :], d["cT"][:, :], reads=[d["cT"]], writes=[cT])
            wst = [fw.sb(es, "mw%d" % i, [128, 8, 512], F32) for i in range(2)]
            mps = [fw.ps(es, "mps%d" % i, [128, 512], F32) for i in range(2)]
            mv = fw.sb(es, "mv", [128, 3072], F32)
            mb = fw.sb(es, "mb", [128, 3072], F32)
            nw = fw.sb(es, "nw", [128, 2048], F32)
            res = fw.sb(es, "mres", [128, 3072], F32)
            i = 0
            for ls in range(4):
                l, s = ls // 2, ls % 2
                self.load_bcast(mb, d["mod_b"], d["mod_b"][ls:ls + 1, :])
                self.load_bcast(nw, d["norm_w"], d["norm_w"][ls:ls + 1, :])
                wv = d["mod_w"].ap()[ls * 1024:(ls + 1) * 1024, :].rearrange("(k p) c -> p k c", p=128)
                for nt in range(6):
                    st = wst[i % 2]
                    ps = mps[i % 2]
                    i += 1
                    fw.dma("sp", st[:, :, :], wv[:, :, nt * 512:(nt + 1) * 512], reads=[d["mod_w"]], writes=[st])
                    for k in range(8):
                        fw.op("pe", lambda: nc.tensor.matmul(ps[:, :], cT[:, k:k + 1].to_broadcast([128, 128]), st[:, k, :], start=(k == 0), stop=(k == 7)), [cT, st], [ps])
                    fw.op("dve", lambda: nc.vector.tensor_tensor(mv[:, nt * 512:(nt + 1) * 512], ps[:, :], mb[:, nt * 512:(nt + 1) * 512], ALU.add), [ps, mb], [mv])
                fw.op("dve", lambda: nc.vector.scalar_tensor_tensor(res[:, 0:1024], mv[:, 1024:2048], 1.0, nw[:, 0:1024], ALU.add, ALU.mult), [mv, nw], [res])
                fw.op("pool", lambda: nc.gpsimd.tensor_copy(res[:, 1024:2048], mv[:, 0:1024]), [mv], [res])
                fw.op("dve", lambda: nc.vector.tensor_tensor(res[:, 2048:3072], mv[:, 2048:3072], nw[:, 1024:2048], ALU.mult), [mv, nw], [res])
                fw.dma("pool", d["MODV"][ls:ls + 1, :], res[0:1, :], reads=[res], writes=[d["MODV"]])
        fw.barrier()

    def load_modv(self, es, ls, which):
        d = self.dram
        out = []
        for i, nm in enumerate("ABG"):
            if nm in which:
                t = self.fw.sb(es, "mod%s" % nm, [128, 1024], F32)
                self.load_bcast(t, d["MODV"], d["MODV"][ls:ls + 1, i * 1024:(i + 1) * 1024])
                out.append(t)
        return out

    def phase_proj(self, l, xin):
        fw, nc = self.fw, self.nc
        d = self.dram
        if l == 0:
            src = d["w_in_ab"]
            fm = [(0, 128, False), (128, 128, False), (256, 128, False), (384, 128, False),
                  (1304, 128, False), (1432, 128, False), (1560, 128, False), (1688, 128, False),
                  (512, 128, False), (640, 128, False),
                  (768, 64, True), (832, 64, True), (1024, 64, True), (1088, 64, True),
                  (1816, 64, True), (1880, 64, True)]
            tm = [(896, 128), (1152, 128), (1944, 128), (1280, 24)]
            FM, TM = d["FM0"], d["TM0"]
            f32_chunks = {}
        else:
            src = d["w_in_cd"]
            fm = [(0, 128, False), (128, 128, False), (256, 128, False), (384, 128, False),
                  (512, 64, True), (576, 64, True),
                  (768, 128, False), (896, 128, False), (1024, 128, False), (1152, 128, False),
                  (1280, 128, False), (1408, 128, False), (1536, 128, False), (1664, 128, False)]
            tm = [(640, 128), (1792, 512)]
            FM, TM = d["FM1"], d["TM1"]
            f32_chunks = {0: 0, 1: 1, 2: 2, 3: 3, 4: 4, 5: 5}
        nfm = len(fm)
        ntm = sum(c for _, c in tm)
        with ExitStack() as es:
            self.alloc_wstage(es)
            Wfm = fw.sb(es, "Wfm", [128, 8, nfm * 128], BF16)
            Wtm = fw.sb(es, "Wtm", [128, 8, ntm], BF16)
            for ci, (c0, ncl, dup) in enumerate(fm):
                self.load_w(es, Wfm, ci * 128, src, 0, c0, ncl, 8, "fm", dup=dup)
            o = 0
            for (c0, ncl) in tm:
                self.load_w(es, Wtm, o, src, 0, c0, ncl, 8, "tm")
                o += ncl
            A, Bv = self.load_modv(es, 2 * l, "AB")
            xts = [fw.sb(es, "xt%d" % i, [128, 1024], F32) for i in range(2)]
            junk = fw.sb(es, "junk", [128, 1024], F32)
            h32 = fw.sb(es, "h32", [128, 1024], F32)
            hbs = [fw.sb(es, "hb%d" % i, [128, 1024], BF16) for i in range(2)]
            smalls = [[fw.sb(es, "sm%d_%d" % (i, j), [128, 1], F32) for j in range(3)] for i in range(2)]
            hTs = [fw.sb(es, "hT%d" % i, [128, 8, 512], BF16) for i in range(2)]
            tps = [fw.ps(es, "tp%d" % i, [128, 1024], BF16) for i in range(2)]
            fps = [fw.ps(es, "fps%d" % i, [128, 512], F32) for i in range(3)]
            tmps_ = [fw.ps(es, "tmps%d" % i, [128, 512], F32) for i in range(2)]
            stg = [fw.sb(es, "stg%d" % i, [128, 512], BF16) for i in range(4)]
            stgf = [fw.sb(es, "stgf%d" % i, [128, 512], F32) for i in range(2)]
            stt = [fw.sb(es, "stt%d" % i, [128, ntm], BF16) for i in range(2)]
            ti = 0
            si = 0
            for st_ in range(8):
                hT = hTs[st_ % 2]
                for tt in range(4):
                    t = st_ * 4 + tt
                    xt = xts[t % 2]
                    fw.dma("sp", xt[:, :], xin[t * 128:(t + 1) * 128, :], reads=[xin], writes=[xt])
                    sm = smalls[t % 2]
                    self.norm_mod_T(xt, A, Bv, hT, hT, tt * 128, (junk, sm[0], sm[1], sm[2], h32, hbs[t % 2]), tps[t % 2])
                for ci in range(nfm):
                    ps = fps[ci % 3]
                    for k in range(8):
                        fw.op("pe", lambda: nc.tensor.matmul(ps[:, :], Wfm[:, k, ci * 128:(ci + 1) * 128], hT[:, k, :], start=(k == 0), stop=(k == 7)), [Wfm, hT], [ps])
                    sg = stg[si % 4]
                    si += 1
                    if ci in f32_chunks:
                        sf = stgf[ci % 2]
                        fw.op("dve", lambda: nc.vector.tensor_copy(sf[:, :], ps[:, :]), [ps], [sf])
                        fw.op("act", lambda: nc.scalar.copy(sg[:, :], sf[:, :]), [sf], [sg])
                        fi = f32_chunks[ci]
                        fw.dma("sp", d["FMF"][fi * 128:(fi + 1) * 128, st_ * 512:(st_ + 1) * 512], sf[:, :], reads=[sf], writes=[d["FMF"]])
                    elif ci % 2 == 0:
                        fw.op("act", lambda: nc.scalar.copy(sg[:, :], ps[:, :]), [ps], [sg])
                    else:
                        fw.op("dve", lambda: nc.vector.tensor_copy(sg[:, :], ps[:, :]), [ps], [sg])
                    fw.dma("pool", FM[ci * 128:(ci + 1) * 128, st_ * 512:(st_ + 1) * 512], sg[:, :], reads=[sg], writes=[FM])
                for tt in range(4):
                    t = st_ * 4 + tt
                    so = stt[t % 2]
                    for c0 in range(0, ntm, 512):
                        w = min(512, ntm - c0)
                        ps = tmps_[ti % 2]
                        ti += 1
                        for k in range(8):
                            fw.op("pe", lambda: nc.tensor.matmul(ps[:, 0:w], hT[:, k, tt * 128:(tt + 1) * 128], Wtm[:, k, c0:c0 + w], start=(k == 0), stop=(k == 7)), [hT, Wtm], [ps])
                        fw.op("dve", lambda: nc.vector.tensor_copy(so[:, c0:c0 + w], ps[:, 0:w]), [ps], [so])
                    fw.dma("pool", TM[t * 128:(t + 1) * 128, :], so[:, :], reads=[so], writes=[TM])
        fw.barrier()

    def phase_outproj(self, l, xin, xout):
        fw, nc = self.fw, self.nc
        d = self.dram
        src = d["w_out_ab"] if l == 0 else d["w_out_cd"]
        O = d["O%d" % l]
        with ExitStack() as es:
            self.alloc_wstage(es)
            Wo = fw.sb(es, "Wo", [128, 8, 1024], BF16)
            for c in range(0, 1024, 256):
                self.load_w(es, Wo, c, src, 0, c, 256, 8, "wo")
            (Gv,) = self.load_modv(es, 2 * l, "G")
            ots = [fw.sb(es, "ot%d" % i, [128, 1024], BF16) for i in range(2)]
            oTs = [fw.sb(es, "oT%d" % i, [128, 8, 128], BF16) for i in range(2)]
            xts = [fw.sb(es, "xt%d" % i, [128, 1024], F32) for i in range(2)]
            xns = [fw.sb(es, "xn%d" % i, [128, 1024], F32) for i in range(2)]
            junk = fw.sb(es, "junk", [128, 1024], F32)
            t32 = fw.sb(es, "t32", [128, 1024], F32)
            smalls = [[fw.sb(es, "sm%d_%d" % (i, j), [128, 2 if j == 0 else 1], F32) for j in range(4)] for i in range(2)]
            tps = [fw.ps(es, "tp%d" % i, [128, 1024], BF16) for i in range(2)]
            yps = [[fw.ps(es, "y%d_%d" % (i, h), [128, 512], F32) for h in range(2)] for i in range(2)]
            for t in range(NT):
                ot, oT, xt, xn = ots[t % 2], oTs[t % 2], xts[t % 2], xns[t % 2]
                fw.dma("sp", ot[:, :], O[t * 128:(t + 1) * 128, :], reads=[O], writes=[ot])
                fw.dma("sp", xt[:, :], xin[t * 128:(t + 1) * 128, :], reads=[xin], writes=[xt])
                tp = tps[t % 2]
                for k in range(8):
                    fw.op("pe", lambda: nc.tensor.transpose(tp[:, k * 128:(k + 1) * 128], ot[:, k * 128:(k + 1) * 128], self.identb[:, :]), [ot, self.identb], [tp])
                fw.op("dve", lambda: nc.vector.tensor_copy(oT[:, :, :], tp[:, :].rearrange("p (k c) -> p k c", k=8)), [tp], [oT])
                yp = yps[t % 2]
                for h in range(2):
                    for k in range(8):
                        fw.op("pe", lambda: nc.tensor.matmul(yp[h][:, :], oT[:, k, :], Wo[:, k, h * 512:(h + 1) * 512], start=(k == 0), stop=(k == 7)), [oT, Wo], [yp[h]])
                sm = smalls[t % 2]
                self.post_norm_residual(yp, xt, Gv, xn, (junk, sm[0], sm[1], sm[2], sm[3], t32))
                fw.dma("pool", xout[t * 128:(t + 1) * 128, :], xn[:, :], reads=[xn], writes=[xout])
        fw.barrier()

    def phase_ffn(self, l, xin, xout):
        fw, nc = self.fw, self.nc
        d = self.dram
        win, wout = d["ffn_w_in"], d["ffn_w_out"]
        with ExitStack() as es:
            self.alloc_wstage(es)
            Wi = fw.sb(es, "Wi", [128, 8, 2 * DFF], BF16)
            Wo = fw.sb(es, "Wo2", [128, NFC, 1024], BF16)
            A, Bv, Gv = self.load_modv(es, 2 * l + 1, "ABG")
            xts = [fw.sb(es, "xt%d" % i, [128, 1024], F32) for i in range(2)]
            xns = [fw.sb(es, "xn%d" % i, [128, 1024], F32) for i in range(2)]
            junk = fw.sb(es, "junk", [128, 1024], F32)
            h32 = fw.sb(es, "h32", [128, 1024], F32)
            hb = fw.sb(es, "hb", [128, 1024], BF16)
            smalls = [[fw.sb(es, "sm%d_%d" % (i, j), [128, 2 if j == 3 else 1], F32) for j in range(7)] for i in range(2)]
            hTs = [fw.sb(es, "hT%d" % i, [128, 8, 256], BF16) for i in range(2)]
            aTs = [fw.sb(es, "aT%d" % i, [128, 256], BF16) for i in range(3)]
            sgs = [fw.sb(es, "sg%d" % i, [128, 256], F32) for i in range(2)]
            tp = fw.ps(es, "tp", [128, 1024], BF16)
            gu = [fw.ps(es, "gu%d" % i, [128, 512], F32) for i in range(2)]
            yps = [[fw.ps(es, "y%d_%d" % (i, h), [128, 512], F32) for h in range(2)] for i in range(2)]
            for c in range(0, 2 * DFF, 256):
                self.load_w(es, Wi, c, win, l * 1024, c, 256, 8, "wi")
            for k0 in range(0, NFC, 2):
                srcv = wout.ap()[l * DFF + k0 * 128: l * DFF + (k0 + 2) * 128, :].rearrange("(k p) c -> p k c", p=128)
                st = self.wstage[self.wstage_i % 2]
                self.wstage_i += 1
                stv = st[:, :].rearrange("p (k c) -> p k c", k=2)
                fw.dma("sp", stv, srcv, reads=[wout], writes=[st])
                e = ["pool", "dve"][self.wstage_i % 2]
                eng = fw.eng[e]
                fw.op(e, lambda: eng.tensor_copy(Wo[:, k0:k0 + 2, :], stv), [st], [Wo])
            ai = 0
            for st_ in range(16):
                hT = hTs[st_ % 2]
                for tt in range(2):
                    t = st_ * 2 + tt
                    xt = xts[tt]
                    fw.dma("sp", xt[:, :], xin[t * 128:(t + 1) * 128, :], reads=[xin], writes=[xt])
                    sm = smalls[tt]
                    self.norm_mod_T(xt, A, Bv, hT, hT, tt * 128, (junk, sm[0], sm[1], sm[2], h32, hb), tp)
                for f in range(NFC):
                    g = gu[f % 2]
                    for half, c0 in ((0, f * 128), (1, DFF + f * 128)):
                        for k in range(8):
                            fw.op("pe", lambda: nc.tensor.matmul(g[:, half * 256:(half + 1) * 256], Wi[:, k, c0:c0 + 128], hT[:, k, :], start=(k == 0), stop=(k == 7)), [Wi, hT], [g])
                    sg = sgs[f % 2]
                    aT = aTs[ai % 3]
                    ai += 1
                    fw.op("act", lambda: nc.scalar.activation(sg[:, :], g[:, 0:256], AF.Silu), [g], [sg])
                    fw.op("dve", lambda: nc.vector.tensor_tensor(aT[:, :], sg[:, :], g[:, 256:512], ALU.mult), [sg, g], [aT])
                    for tt in range(2):
                        for h in range(2):
                            fw.op("pe", lambda: nc.tensor.matmul(yps[tt][h][:, :], aT[:, tt * 128:(tt + 1) * 128], Wo[:, f, h * 512:(h + 1) * 512], start=(f == 0), stop=(f == NFC - 1)), [aT, Wo], [yps[tt][h]])
                for tt in range(2):
                    t = st_ * 2 + tt
                    sm = smalls[tt]
                    xn = xns[tt]
                    self.post_norm_residual(yps[tt], xts[tt], Gv, xn, (junk, sm[3], sm[4], sm[5], sm[6], h32))
                    fw.dma("pool", xout[t * 128:(t + 1) * 128, :], xn[:, :], reads=[xn], writes=[xout])
        fw.barrier()

    def build_bias_tables(self, es, tabs_t, cst_t, base_pos, use_swa_mask):
        fw, nc = self.fw, self.nc
        T0 = fw.sb(es, "T0", [128, 8, 128], F32)
        T1 = fw.sb(es, "T1", [128, 8, 128], F32)
        for pos in range(8):
            gp = base_pos + pos
            b31 = tabs_t[:, T_B31 + gp:T_B31 + gp + 1]
            toe0 = tabs_t[:, T_TOE0 + gp * 128:T_TOE0 + (gp + 1) * 128]
            toe1 = tabs_t[:, T_TOE1 + gp * 128:T_TOE1 + (gp + 1) * 128]
            fw.op("dve", lambda: nc.vector.scalar_tensor_tensor(T0[:, pos, :], toe0, b31, cst_t[:, C_MASK0:C_MASK0 + 128], ALU.subtract, ALU.add), [tabs_t, cst_t], [T0])
            if use_swa_mask:
                fw.op("dve", lambda: nc.vector.scalar_tensor_tensor(T1[:, pos, :], toe1, b31, cst_t[:, C_MASKLT:C_MASKLT + 128], ALU.subtract, ALU.add), [tabs_t, cst_t], [T1])
            else:
                fw.op("dve", lambda: nc.vector.tensor_scalar(T1[:, pos, :], toe1, b31, None, ALU.subtract), [tabs_t], [T1])
        return T0, T1

    def load_tabs(self, es):
        fw = self.fw
        d = self.dram
        tabs_t = fw.sb(es, "tabs_t", [128, T_END], F32)
        cst_t = fw.sb(es, "cst_t", [128, C_END], F32)
        fw.dma("sp", tabs_t[:, :], d["tabs"][:, :], reads=[d["tabs"]], writes=[tabs_t])
        fw.dma("sp", cst_t[:, :], d["consts"][:, :], reads=[d["consts"]], writes=[cst_t])
        return tabs_t, cst_t

    def score_tile(self, ps, KT, g, kt_lo, kt_n, QT, j, mask=None):
        fw, nc = self.fw, self.nc
        first = True
        if mask is not None:
            E, NEGT = mask
            fw.op("pe", lambda: nc.tensor.matmul(ps[0:kt_n, 0:512], E, NEGT[0:64, :, :], start=True, stop=False, skip_group_check=True), [NEGT, self.ebig_t], [ps])
            first = False
        for half in range(2):
            pr = slice(64 * half, 64 * half + 64)
            fw.op("pe", lambda: nc.tensor.matmul(ps[0:kt_n, half * 256:(half + 1) * 256], KT[pr, g, kt_lo:kt_lo + kt_n],
                                                 QT[pr, 2 * g:2 * g + 2, j * 128:(j + 1) * 128], start=first, stop=True, skip_group_check=True),
                  [KT, QT], [ps])

    def exp_tile(self, ps, PT, rows, table=None, tmp=None):
        fw, nc = self.fw, self.nc
        if table is None:
            fw.op("act", lambda: nc.scalar.activation(PT[0:rows, :], ps[0:rows, :], AF.Exp, scale=0.125), [ps], [PT])
        else:
            tb, tap = table
            fw.op("dve", lambda: nc.vector.scalar_tensor_tensor(tmp[0:rows, :].rearrange("p (s q) -> p s q", s=4), ps[0:rows, :].rearrange("p (s q) -> p s q", s=4),
                                                                0.125, tap, ALU.mult, ALU.add), [ps, tb], [tmp])
            fw.op("act", lambda: nc.scalar.activation(PT[0:rows, :], tmp[0:rows, :], AF.Exp), [tmp], [PT])

    def phase_mix0(self):
        fw, nc = self.fw, self.nc
        d = self.dram
        FM, TM, O = d["FM0"], d["TM0"], d["O0"]
        tmv = TM.ap().rearrange("(t p) c -> p t c", p=128)
        with ExitStack() as es:
            tabs_t, cst_t = self.load_tabs(es)
            self.ebig_t = fw.sb(es, "ebig_t", [128, 4096], BF16)
            zeros = fw.sb(es, "zeros", [128, 512], BF16)
            fw.op("pool", lambda: nc.gpsimd.memset(zeros[:, :], 0.0), [], [zeros])
            W2 = fw.sb(es, "W2", [128, 32, 192], BF16)
            peT = fw.sb(es, "peT", [128, 32], BF16)
            KCT = fw.sb(es, "KCT", [128, 2, 256], BF16)
            with ExitStack() as es2:
                wst = fw.sb(es2, "cwst", [128, 32, 128], F32)
                pst = fw.sb(es2, "pst", [128, 32], F32)
                est = fw.sb(es2, "est", [128, 4096], F32)
                cw = d["cmp_w"].ap().rearrange("(l d) e -> d l e", d=64)
                for hf in range(2):
                    fw.dma("sp", wst[64 * hf:64 * hf + 64, :, :], cw, reads=[d["cmp_w"]], writes=[wst])
                    fw.dma("sp", pst[64 * hf:64 * hf + 64, :], d["cmp_pe"][:, :], reads=[d["cmp_pe"]], writes=[pst])
                fw.op("dve", lambda: nc.vector.tensor_copy(W2[:, :, 0:64], wst[:, :, 0:64]), [wst], [W2])
                fw.op("dve", lambda: nc.vector.tensor_copy(W2[:, :, 64:128], wst[:, :, 0:64]), [wst], [W2])
                fw.op("dve", lambda: nc.vector.tensor_copy(W2[:, :, 128:192], wst[:, :, 64:128]), [wst], [W2])
                fw.op("dve", lambda: nc.vector.tensor_copy(peT[:, :], pst[:, :]), [pst], [peT])
                fw.dma("sp", est[:, :], d["ebig"][:, :], reads=[d["ebig"]], writes=[est])
                fw.op("pool", lambda: nc.gpsimd.tensor_copy(self.ebig_t[:, :], est[:, :]), [est], [self.ebig_t])
                KCR = fw.sb(es2, "KCR", [128, 4096], BF16)
                VCR = fw.sb(es2, "VCR", [128, 4096], BF16)
                fw.dma("sp", KCR[:, :], FM[8 * 128:9 * 128, :], reads=[FM], writes=[KCR])
                fw.dma("sp", VCR[:, :], FM[9 * 128:10 * 128, :], reads=[FM], writes=[VCR])
                kps = fw.ps(es2, "kps", [128, 512], F32)
                kpe = fw.sb(es2, "kpe", [128, 2], F32)
                VCE = fw.sb(es2, "VCE", [128, 2, 2, 128], BF16)
                fw.op("pool", lambda: nc.gpsimd.memset(VCE[:, :, :, :], 0.0), [], [VCE])
                for g in range(2):
                    pr = slice(64 * g, 64 * g + 64)
                    for l in range(32):
                        fw.op("pe", lambda: nc.tensor.matmul(kps[:, 300:301], W2[pr, l, 0:128], peT[pr, l:l + 1], start=(l == 0), stop=(l == 31)), [W2, peT], [kps])
                    fw.op("dve", lambda: nc.vector.tensor_copy(kpe[:, g:g + 1], kps[:, 300:301]), [kps], [kpe])
                    for l in range(32):
                        fw.op("pe", lambda: nc.tensor.matmul(kps[:, 0:255], W2[pr, l, 0:128], KCR[pr, l:l + 16 * 254 + 1:16], start=(l == 0), stop=(l == 31)), [W2, KCR], [kps])
                    fw.op("dve", lambda: nc.vector.tensor_scalar(KCT[:, g, 0:255], kps[:, 0:255], kpe[:, g:g + 1], None, ALU.add), [kps, kpe], [KCT])
                    for ti in range(2):
                        nr = 128 if ti == 0 else 127
                        t0 = 16 * 128 * ti
                        for l in range(32):
                            fw.op("pe", lambda: nc.tensor.matmul(kps[0:nr, 384:448], VCR[pr, t0 + l:t0 + l + 16 * (nr - 1) + 1:16], W2[pr, l, 128:192], start=(l == 0), stop=False), [W2, VCR], [kps])
                        for l in range(32):
                            fw.op("pe", lambda: nc.tensor.matmul(kps[0:nr, 384:448], peT[pr, l:l + 1].to_broadcast([64, nr]), W2[pr, l, 128:192], start=False, stop=(l == 31)), [W2, peT], [kps])
                        fw.op("dve", lambda: nc.vector.tensor_copy(VCE[0:nr, ti, g, 0:64], kps[0:nr, 384:448]), [kps], [VCE])
                        fw.op("dve", lambda: nc.vector.tensor_copy(VCE[0:nr, ti, g, 64:128], cst_t[0:nr, C_OVW + ti * 64:C_OVW + (ti + 1) * 64]), [cst_t], [VCE])
                fw.dma("pool", d["VCD"].ap().rearrange("(t p) c -> p t c", p=128), VCE[:, :, :, :].rearrange("p t g c -> p t (g c)"), reads=[VCE], writes=[d["VCD"]])
                if "KCTD" in self.dbg:
                    fw.dma("pool", d["KCTD"][:, :], KCT[:, :, :].rearrange("p g n -> p (g n)"), reads=[KCT], writes=[d["KCTD"]])
            fw.barrier()
            VCF = fw.sb(es, "VCF", [128, 2, 256], BF16)
            VCN = fw.sb(es, "VCN", [16, NT, 256], BF16)
            fw.dma("sp", VCF[:, :, :], d["VCD"].ap().rearrange("(t p) c -> p t c", p=128), reads=[d["VCD"]], writes=[VCF])
            for j in range(NT):
                n0 = max(0, 8 * j - 9)
                nn = 8 * j + 7 - n0
                fw.dma("sp", VCN[0:nn, j, :], d["VCD"][n0:n0 + nn, :], reads=[d["VCD"]], writes=[VCN])
            QA = fw.sb(es, "QA", [128, 4, S], BF16)
            KS = fw.sb(es, "KS", [128, 2, S], BF16)
            KW = fw.sb(es, "KW", [128, 2, S], BF16)
            for c in range(4):
                fw.dma("sp", QA[:, c, :], FM[c * 128:(c + 1) * 128, :], reads=[FM], writes=[QA])
            for g in range(2):
                fw.dma("sp", KS[:, g, :], FM[(10 + g) * 128:(11 + g) * 128, :], reads=[FM], writes=[KS])
                fw.dma("sp", KW[:, g, :], FM[(12 + g) * 128:(13 + g) * 128, :], reads=[FM], writes=[KW])
            VS = fw.sb(es, "VS", [128, NT, 2, 65], BF16)
            VW = fw.sb(es, "VW", [128, NT, 2, 65], BF16)
            fw.op("pool", lambda: nc.gpsimd.memset(VS[:, :, :, :], 1.0), [], [VS])
            fw.op("pool", lambda: nc.gpsimd.memset(VW[:, :, :, :], 1.0), [], [VW])
            for g in range(2):
                fw.dma("sp", VS[:, :, g, 0:64], tmv[:, :, g * 64:(g + 1) * 64], reads=[TM], writes=[VS])
                fw.dma("sp", VW[:, :, g, 0:64], tmv[:, :, 128 + g * 64:128 + (g + 1) * 64], reads=[TM], writes=[VW])
            GS2 = fw.sb(es, "GS2", [128, NT, 2, 3, 4], F32)
            with ExitStack() as es2:
                GAb = fw.sb(es2, "GAb", [128, NT, 24], BF16)
                GAf = fw.sb(es2, "GAf", [128, NT, 24], F32)
                fw.dma("sp", GAb[:, :, :], tmv[:, :, 384:408], reads=[TM], writes=[GAb])
                fw.op("act", lambda: nc.scalar.activation(GAf[:, :, :], GAb[:, :, :], AF.Exp, scale=-1.0), [GAb], [GAf])
                fw.op("dve", lambda: nc.vector.tensor_scalar(GAf[:, :, :], GAf[:, :, :], 1.0, None, ALU.add), [GAf], [GAf])
                fw.op("dve", lambda: nc.vector.reciprocal(GAf[:, :, :], GAf[:, :, :]), [GAf], [GAf])
                for g in range(2):
                    for slot in range(4):
                        h = 4 * g + PERM[slot]
                        fw.op("dve", lambda: nc.vector.tensor_copy(GS2[:, :, g, :, slot], GAf[:, :, 3 * h:3 * h + 3]), [GAf], [GS2])
            fw.barrier()
            TA0, TA1 = self.build_bias_tables(es, tabs_t, cst_t, 0, False)
            TCs = []
            for nm, toff, moff in (("TCG", T_TCG, C_MCG), ("TC0", T_TC0, C_MC0), ("TC1", T_TC1, C_MC1)):
                tct = fw.sb(es, nm, [16, 8, 128], F32)
                for pos in range(8):
                    fw.op("dve", lambda: nc.vector.scalar_tensor_tensor(tct[0:16, pos, :], tabs_t[0:16, toff + pos * 128:toff + (pos + 1) * 128], tabs_t[0:16, T_B31 + pos:T_B31 + pos + 1],
                                                                        cst_t[0:16, moff:moff + 128], ALU.subtract, ALU.add), [tabs_t, cst_t], [tct])
                TCs.append(tct)
            MLT = fw.view(cst_t, "mlt")
            sc = [fw.ps(es, "sc%d" % i, [128, 512], F32) for i in range(3)]
            cps = fw.ps(es, "cps", [128, 512], F32)
            sps = fw.ps(es, "sps", [128, 512], F32)
            wps = fw.ps(es, "wps", [128, 512], F32)
            tps = fw.ps(es, "tps", [128, 1024], BF16)
            PTs = [fw.sb(es, "PT%d" % i, [128, 512], BF16) for i in range(6)]
            tmps = [fw.sb(es, "tmp%d" % i, [128, 512], F32) for i in range(3)]
            Fm = fw.sb(es, "Fm", [128, 64], F32)
            sm = {nm: [fw.sb(es, "%s%d" % (nm, i), shp, F32) for i in range(2)] for nm, shp in
                  (("den", [128, 4]), ("rden", [128, 4]), ("coef", [128, 4]), ("imp", [128, 64]), ("val", [128, 64]), ("m8", [128, 8]), ("thr", [128, 1]))}
            nmk = [fw.sb(es, "nmk%d" % i, [128, 64], BF16) for i in range(2)]
            NEGTs = [fw.sb(es, "NEGT%d" % i, [64, 4, 128], BF16) for i in range(2)]
            OAs = [fw.sb(es, "OA%d" % i, [128, 8, 64], F32) for i in range(2)]
            OBs = [fw.sb(es, "OAb%d" % i, [128, 512], BF16) for i in range(2)]
            cnt = {"sc": 0, "pt": 0, "tmp": 0, "u": 0}

            def nxt(key, lst):
                v = lst[cnt[key] % len(lst)]
                cnt[key] += 1
                return v

            def finish_branch(ps_acc, width, den_ap, br, j, g, OA, first):
                u = cnt["u"] % 2
                cnt["u"] += 1
                den, rden, coef = sm["den"][u], sm["rden"][u], sm["coef"][u]
                fw.op("dve", lambda: nc.vector.tensor_scalar(den[:, :], den_ap, 1e-30, None, ALU.max), [ps_acc], [den])
                fw.op("dve", lambda: nc.vector.reciprocal(rden[:, :], den[:, :]), [den], [rden])
                fw.op("dve", lambda: nc.vector.tensor_tensor(coef[:, :], rden[:, :], GS2[:, j, g, br, :], ALU.mult), [rden, GS2], [coef])
                for slot in range(4):
                    h = 4 * g + PERM[slot]
                    src = ps_acc[:, slot * width:slot * width + 64]
                    if first:
                        fw.op("dve", lambda: nc.vector.tensor_scalar(OA[:, h, :], src, coef[:, slot:slot + 1], None, ALU.mult), [ps_acc, coef], [OA])
                    else:
                        fw.op("dve", lambda: nc.vector.scalar_tensor_tensor(OA[:, h, :], src, coef[:, slot:slot + 1], OA[:, h, :], ALU.mult, ALU.add), [ps_acc, coef, OA], [OA])
                return rden

            for j in range(NT):
                OA = OAs[j % 2]
                fw.op("pool", lambda: nc.gpsimd.memset(Fm[:, :], 0.0), [], [Fm])
                fw.op("pool", lambda: nc.gpsimd.memset(Fm[:, 0:1], 1e4), [], [Fm])
                if j >= 1:
                    fw.op("pool", lambda: nc.gpsimd.memset(Fm[0:64, 2 * j - 1:2 * j + 1], 1e4), [], [Fm])
                fw.op("pool", lambda: nc.gpsimd.memset(Fm[0:64, 2 * j + 1:64], -1e30), [], [Fm])
                fw.op("pool", lambda: nc.gpsimd.memset(Fm[64:128, 2 * j:2 * j + 2], 1e4), [], [Fm])
                if 2 * j + 2 < 64:
                    fw.op("pool", lambda: nc.gpsimd.memset(Fm[64:128, 2 * j + 2:64], -1e30), [], [Fm])
                n0 = max(0, 8 * j - 9)
                nn = 8 * j + 7 - n0
                TC = TCs[1] if j == 0 else (TCs[2] if j == 1 else TCs[0])
                for g in range(2):
                    tiles = []
                    lo = 0
                    while lo < n0:
                        r = min(128, n0 - lo)
                        tiles.append((lo, r, "far"))
                        lo += r
                    tiles.append((n0, nn, "near"))
                    pts = []
                    for (lo, r, kind) in tiles:
                        ps = nxt("sc", sc)
                        self.score_tile(ps, KCT, g, lo, r, QA, j)
                        PT = nxt("pt", PTs)
                        if kind == "far":
                            self.exp_tile(ps, PT, r)
                        else:
                            self.exp_tile(ps, PT, r, (TC, TC[0:r, 4 * g:4 * g + 4, :]), nxt("tmp", tmps))
                        pts.append(PT)
                    for slot in range(4):
                        for i, (lo, r, kind) in enumerate(tiles):
                            rhs = VCF[0:r, lo // 128, g * 128:(g + 1) * 128] if kind == "far" else VCN[0:r, j, g * 128:(g + 1) * 128]
                            rb = VCF if kind == "far" else VCN
                            fw.op("pe", lambda: nc.tensor.matmul(cps[:, slot * 128:(slot + 1) * 128], pts[i][0:r, slot * 128:(slot + 1) * 128], rhs,
                                                                 start=(i == 0), stop=(i == len(tiles) - 1)), [pts[i], rb], [cps])
                    u = cnt["u"] % 2
                    den = sm["den"][u]
                    fw.op("dve", lambda: nc.vector.tensor_reduce(den[:, :], cps[:, :].rearrange("p (s c) -> p s c", s=4)[:, :, 64:128], AX.X, ALU.add), [cps], [den])
                    rden = finish_branch(cps, 128, den[:, :], 0, j, g, OA, True)
                    imp, val, m8, thr = sm["imp"][u], sm["val"][u], sm["m8"][u], sm["thr"][u]
                    fw.op("dve", lambda: nc.vector.tensor_scalar(imp[:, :], cps[:, 64:128], rden[:, 0:1], None, ALU.mult), [cps, rden], [imp])
                    for slot in range(1, 4):
                        fw.op("dve", lambda: nc.vector.scalar_tensor_tensor(imp[:, :], cps[:, slot * 128 + 64:slot * 128 + 128], rden[:, slot:slot + 1], imp[:, :], ALU.mult, ALU.add), [cps, rden, imp], [imp])
                    fw.op("dve", lambda: nc.vector.tensor_tensor(val[:, :], imp[:, :], Fm[:, :], ALU.add), [imp, Fm], [val])
                    fw.op("dve", lambda: nc.vector.max(out=m8[:, :], in_=val[:, :]), [val], [m8])
                    fw.op("dve", lambda: nc.vector.tensor_scalar(thr[:, :], m8[:, 7:8], -1e29, None, ALU.max), [m8], [thr])
                    fw.op("dve", lambda: nc.vector.tensor_scalar(val[:, :], val[:, :], thr[:, 0:1], None, ALU.is_lt), [val, thr], [val])
                    nk = nmk[u]
                    fw.op("dve", lambda: nc.vector.tensor_scalar(nk[:, :], val[:, :], 8.0 * NEG, None, ALU.mult), [val], [nk])
                    fw.op("pe", lambda: nc.tensor.transpose(tps[0:64, 0:128], nk[:, :], self.identb[:, :]), [nk, self.identb], [tps])
                    NEGT = NEGTs[u]
                    fw.op("dve", lambda: nc.vector.tensor_copy(NEGT[0:64, :, :], tps[0:64, 0:128].unsqueeze(1).to_broadcast([64, 4, 128])), [tps], [NEGT])
                    if "SELD" in self.dbg:
                        fw.dma("pool", d["SELD"][j * 128:(j + 1) * 128, g * 64:(g + 1) * 64], nk[:, :], reads=[nk], writes=[d["SELD"]])
                    fw.op("pe", lambda: nc.tensor.matmul(sps[:, 0:260], zeros[:, 0:128], zeros[:, 0:260], start=True, stop=False, skip_group_check=True), [zeros], [sps])
                    for kt in range(j + 1):
                        ps = nxt("sc", sc)
                        self.score_tile(ps, KS, g, kt * 128, 128, QA, j, mask=(self.ebig_t[0:64, kt * 128:(kt + 1) * 128], NEGT))
                        PT = nxt("pt", PTs)
                        if kt == j:
                            self.exp_tile(ps, PT, 128, (TA0, TA0[:, 4 * g:4 * g + 4, :]), nxt("tmp", tmps))
                        elif kt == j - 1:
                            self.exp_tile(ps, PT, 128, (TA1, TA1[:, 4 * g:4 * g + 4, :]), nxt("tmp", tmps))
                        else:
                            self.exp_tile(ps, PT, 128)
                        for slot in range(4):
                            fw.op("pe", lambda: nc.tensor.matmul(sps[:, slot * 65:(slot + 1) * 65], PT[:, slot * 128:(slot + 1) * 128], VS[:, kt, g, :],
                                                                 start=False, stop=(kt == j), skip_group_check=True), [PT, VS], [sps])
                    finish_branch(sps, 65, sps[:, 0:260].rearrange("p (s c) -> p s c", s=4)[:, :, 64], 1, j, g, OA, False)
                    fw.op("pe", lambda: nc.tensor.matmul(wps[:, 0:260], zeros[:, 0:128], zeros[:, 0:260], start=True, stop=False, skip_group_check=True), [zeros], [wps])
                    for kt in range(max(0, j - 4), j + 1):
                        ps = nxt("sc", sc)
                        self.score_tile(ps, KW, g, kt * 128, 128, QA, j)
                        PT = nxt("pt", PTs)
                        if kt == j:
                            self.exp_tile(ps, PT, 128, (TA0, TA0[:, 4 * g:4 * g + 4, :]), nxt("tmp", tmps))
                        elif kt == j - 1:
                            self.exp_tile(ps, PT, 128, (TA1, TA1[:, 4 * g:4 * g + 4, :]), nxt("tmp", tmps))
                        elif kt == j - 4:
                            self.exp_tile(ps, PT, 128, (MLT, cst_t[:, C_MASKLT:C_MASKLT + 128].unsqueeze(1).to_broadcast([128, 4, 128])), nxt("tmp", tmps))
                        else:
                            self.exp_tile(ps, PT, 128)
                        for slot in range(4):
                            fw.op("pe", lambda: nc.tensor.matmul(wps[:, slot * 65:(slot + 1) * 65], PT[:, slot * 128:(slot + 1) * 128], VW[:, kt, g, :],
                                                                 start=False, stop=(kt == j), skip_group_check=True), [PT, VW], [wps])
                    finish_branch(wps, 65, wps[:, 0:260].rearrange("p (s c) -> p s c", s=4)[:, :, 64], 2, j, g, OA, False)
                OB = OBs[j % 2]
                fw.op("pool", lambda: nc.gpsimd.tensor_copy(OB[:, :], OA[:, :, :].rearrange("p h c -> p (h c)")), [OA], [OB])
                fw.dma("pool", O[j * 128:(j + 1) * 128, 0:512], OB[:, :], reads=[OB], writes=[O])
        fw.barrier()
        with ExitStack() as es:
            tabs_t, cst_t = self.load_tabs(es)
            zeros = fw.sb(es, "zeros", [128, 512], BF16)
            fw.op("pool", lambda: nc.gpsimd.memset(zeros[:, :], 0.0), [], [zeros])
            QB = fw.sb(es, "QB", [128, 4, S], BF16)
            KB = fw.sb(es, "KB", [128, 2, S], BF16)
            for c in range(4):
                fw.dma("sp", QB[:, c, :], FM[(4 + c) * 128:(5 + c) * 128, :], reads=[FM], writes=[QB])
            for g in range(2):
                fw.dma("sp", KB[:, g, :], FM[(14 + g) * 128:(15 + g) * 128, :], reads=[FM], writes=[KB])
            VB = fw.sb(es, "VB", [128, NT, 2, 65], BF16)
            fw.op("pool", lambda: nc.gpsimd.memset(VB[:, :, :, :], 1.0), [], [VB])
            for g in range(2):
                fw.dma("sp", VB[:, :, g, 0:64], tmv[:, :, 256 + g * 64:256 + (g + 1) * 64], reads=[TM], writes=[VB])
            TB0, TB1 = self.build_bias_tables(es, tabs_t, cst_t, 8, True)
            snk = fw.sb(es, "snk", [128, 8], F32)
            snkE = fw.sb(es, "snkE", [128, 8], F32)
            self.load_bcast(snk, d["sinks"], d["sinks"][0:1, :])
            for g in range(2):
                for slot in range(4):
                    pos = 4 * g + slot
                    h = 4 * g + PERM[slot]
                    fw.op("dve", lambda: nc.vector.tensor_tensor(snkE[:, pos:pos + 1], snk[:, h:h + 1], tabs_t[:, T_B31 + 8 + pos:T_B31 + 8 + pos + 1], ALU.subtract), [snk, tabs_t], [snkE])
            fw.op("act", lambda: nc.scalar.activation(snkE[:, :], snkE[:, :], AF.Exp), [snkE], [snkE])
            sc = [fw.ps(es, "sc%d" % i, [128, 512], F32) for i in range(3)]
            bps = [fw.ps(es, "bps%d" % i, [128, 512], F32) for i in range(2)]
            PTs = [fw.sb(es, "PT%d" % i, [128, 512], BF16) for i in range(4)]
            tmps = [fw.sb(es, "tmp%d" % i, [128, 512], F32) for i in range(3)]
            dens = [fw.sb(es, "den%d" % i, [128, 4], F32) for i in range(2)]
            OAs = [fw.sb(es, "OB%d" % i, [128, 8, 64], BF16) for i in range(2)]
            c_sc = c_pt = c_tmp = c_u = 0
            for j in range(NT):
                OA = OAs[j % 2]
                for g in range(2):
                    bp = bps[c_u % 2]
                    den = dens[c_u % 2]
                    c_u += 1
                    fw.op("pe", lambda: nc.tensor.matmul(bp[:, 0:260], zeros[:, 0:128], zeros[:, 0:260], start=True, stop=False, skip_group_check=True), [zeros], [bp])
                    for kt in range(max(0, j - 1), j + 1):
                        ps = sc[c_sc % 3]; c_sc += 1
                        self.score_tile(ps, KB, g, kt * 128, 128, QB, j)
                        PT = PTs[c_pt % 4]; c_pt += 1
                        tm_ = tmps[c_tmp % 3]; c_tmp += 1
                        T = TB0 if kt == j else TB1
                        self.exp_tile(ps, PT, 128, (T, T[:, 4 * g:4 * g + 4, :]), tm_)
                        for slot in range(4):
                            fw.op("pe", lambda: nc.tensor.matmul(bp[:, slot * 65:(slot + 1) * 65], PT[:, slot * 128:(slot + 1) * 128], VB[:, kt, g, :],
                                                                 start=False, stop=(kt == j), skip_group_check=True), [PT, VB], [bp])
                    fw.op("dve", lambda: nc.vector.tensor_tensor(den[:, :], bp[:, 0:260].rearrange("p (s c) -> p s c", s=4)[:, :, 64], snkE[:, 4 * g:4 * g + 4], ALU.add), [bp, snkE], [den])
                    fw.op("dve", lambda: nc.vector.reciprocal(den[:, :], den[:, :]), [den], [den])
                    for slot in range(4):
                        h = 4 * g + PERM[slot]
                        fw.op("dve", lambda: nc.vector.tensor_scalar(OA[:, h, :], bp[:, slot * 65:slot * 65 + 64], den[:, slot:slot + 1], None, ALU.mult), [bp, den], [OA])
                fw.dma("pool", O[j * 128:(j + 1) * 128, 512:1024], OA[:, :, :].rearrange("p h c -> p (h c)"), reads=[OA], writes=[O])
        fw.barrier()

    def phase_mix1(self):
        fw, nc = self.fw, self.nc
        d = self.dram
        FM, FMF, TM, O = d["FM1"], d["FMF"], d["TM1"], d["O1"]
        tmv = TM.ap().rearrange("(t p) c -> p t c", p=128)
        with ExitStack() as es:
            tabs_t, cst_t = self.load_tabs(es)
            TA0, TA1 = self.build_bias_tables(es, tabs_t, cst_t, 0, False)
            KMT = fw.sb(es, "KMT", [128, 2, 16], F32)
            with ExitStack() as es2:
                kf = fw.sb(es2, "kf", [128, 4096], F32)
                for g in range(2):
                    fw.dma("sp", kf[:, :], FMF[(4 + g) * 128:(5 + g) * 128, :], reads=[FMF], writes=[kf])
                    fw.op("dve", lambda: nc.vector.tensor_reduce(KMT[:, g, :], kf[:, :].rearrange("p (n l) -> p n l", l=256), AX.X, ALU.add), [kf], [KMT])
                fw.op("dve", lambda: nc.vector.tensor_scalar(KMT[:, :, :], KMT[:, :, :], 1.0 / 256.0, None, ALU.mult), [KMT], [KMT])
            fw.barrier()
            QC = fw.sb(es, "QC", [128, 4, S], BF16)
            KC = fw.sb(es, "KC", [128, 2, S], BF16)
            for c in range(4):
                fw.dma("sp", QC[:, c, :], FM[c * 128:(c + 1) * 128, :], reads=[FM], writes=[QC])
            for g in range(2):
                fw.dma("sp", KC[:, g, :], FM[(4 + g) * 128:(5 + g) * 128, :], reads=[FM], writes=[KC])
            VC = fw.sb(es, "VC", [128, NT, 2, 65], BF16)
            fw.op("pool", lambda: nc.gpsimd.memset(VC[:, :, :, :], 1.0), [], [VC])
            for g in range(2):
                fw.dma("sp", VC[:, :, g, 0:64], tmv[:, :, g * 64:(g + 1) * 64], reads=[TM], writes=[VC])
            sc = [fw.ps(es, "sc%d" % i, [128, 512], F32) for i in range(3)]
            bps = [fw.ps(es, "bps%d" % i, [128, 512], F32) for i in range(2)]
            gps = fw.ps(es, "gps", [128, 512], F32)
            PTs = [fw.sb(es, "PT%d" % i, [128, 512], BF16) for i in range(6)]
            tmps = [fw.sb(es, "tmp%d" % i, [128, 512], F32) for i in range(3)]
            qfs = [fw.sb(es, "qf%d" % i, [128, 4, 128], F32) for i in range(2)]
            OM = fw.sb(es, "OM", [128, 8, 16], F32)
            LT = fw.sb(es, "LT", [128, 8, 16], F32)
            gsm = [fw.sb(es, "gsm%d" % i, [128, 8, 16], F32) for i in range(2)]
            sel = [fw.sb(es, "sel%d" % i, [128, 8, 16], F32) for i in range(2)]
            m8 = [fw.sb(es, "m8%d" % i, [128, 8, 8], F32) for i in range(2)]
            OAs = [fw.sb(es, "OAc%d" % i, [128, 8, 65], F32) for i in range(2)]
            rdn = [fw.sb(es, "rdn%d" % i, [128, 8], F32) for i in range(2)]
            OBs = [fw.sb(es, "OCb%d" % i, [128, 8, 64], BF16) for i in range(2)]
            c_sc = c_pt = c_tmp = c_b = 0
            fmfv = FMF.ap()[0:512, :].rearrange("(c p) q -> p c q", p=128)
            for j in range(NT):
                own = j // 2
                u = j % 2
                if j % 2 == 0:
                    fw.op("pool", lambda: nc.gpsimd.memset(OM[:, :, :], -1e30), [], [OM])
                    fw.op("pool", lambda: nc.gpsimd.memset(LT[:, :, :], 0.0), [], [LT])
                    if own > 0:
                        fw.op("pool", lambda: nc.gpsimd.memset(OM[:, :, 0:own], 0.0), [], [OM])
                        fw.op("pool", lambda: nc.gpsimd.memset(LT[:, :, 0:own], 1.0), [], [LT])
                qf = qfs[u]
                fw.dma("sp", qf[:, :, :], fmfv[:, :, j * 128:(j + 1) * 128], reads=[FMF], writes=[qf])
                for h in range(8):
                    pr = slice(64 * (h % 2), 64 * (h % 2) + 64)
                    fw.op("pe", lambda: nc.tensor.matmul(gps[:, h * 16:(h + 1) * 16], qf[pr, h // 2, :], KMT[pr, h // 4, :], start=True, stop=True), [qf, KMT], [gps])
                G_, S_, M_ = gsm[u], sel[u], m8[u]
                fw.op("dve", lambda: nc.vector.tensor_tensor(G_[:, :, :], gps[:, 0:128].rearrange("p (h n) -> p h n", h=8), OM[:, :, :], ALU.add), [gps, OM], [G_])
                for h in range(8):
                    fw.op("dve", lambda: nc.vector.max(out=M_[:, h, :], in_=G_[:, h, :]), [G_], [M_])
                fw.op("dve", lambda: nc.vector.tensor_tensor(S_[:, :, :], G_[:, :, :], M_[:, :, 2:3].to_broadcast([128, 8, 16]), ALU.is_ge), [G_, M_], [S_])
                fw.op("dve", lambda: nc.vector.tensor_tensor(S_[:, :, :], S_[:, :, :], LT[:, :, :], ALU.mult), [S_, LT], [S_])
                fw.op("dve", lambda: nc.vector.memset(S_[:, :, own:own + 1], 1.0), [], [S_])
                if "GSM1" in self.dbg:
                    if not dict.__contains__(d, "GSM1"):
                        self.dscr("GSM1", [S, 128], F32)
                    fw.dma("pool", d["GSM1"][j * 128:(j + 1) * 128, :], G_[:, :, :].rearrange("p h n -> p (h n)"), reads=[G_], writes=[d["GSM1"]])
                if "SEL1" in self.dbg:
                    fw.dma("pool", d["SEL1"][j * 128:(j + 1) * 128, :], S_[:, :, :].rearrange("p h n -> p (h n)"), reads=[S_], writes=[d["SEL1"]])
                OA = OAs[u]
                for g in range(2):
                    for n in range(own + 1):
                        kts = [kt for kt in (2 * n, 2 * n + 1) if kt <= j]
                        pts = []
                        for kt in kts:
                            ps = sc[c_sc % 3]; c_sc += 1
                            self.score_tile(ps, KC, g, kt * 128, 128, QC, j)
                            PT = PTs[c_pt % 6]; c_pt += 1
                            if kt == j:
                                tm_ = tmps[c_tmp % 3]; c_tmp += 1
                                self.exp_tile(ps, PT, 128, (TA0, TA0[:, 4 * g:4 * g + 4, :]), tm_)
                            elif kt == j - 1:
                                tm_ = tmps[c_tmp % 3]; c_tmp += 1
                                self.exp_tile(ps, PT, 128, (TA1, TA1[:, 4 * g:4 * g + 4, :]), tm_)
                            else:
                                self.exp_tile(ps, PT, 128)
                            pts.append(PT)
                        bp = bps[c_b % 2]; c_b += 1
                        for slot in range(4):
                            for i, kt in enumerate(kts):
                                fw.op("pe", lambda: nc.tensor.matmul(bp[:, slot * 65:(slot + 1) * 65], pts[i][:, slot * 128:(slot + 1) * 128], VC[:, kt, g, :],
                                                                     start=(i == 0), stop=(i == len(kts) - 1)), [pts[i], VC], [bp])
                        for slot in range(4):
                            h = 4 * g + PERM[slot]
                            if n == 0:
                                fw.op("dve", lambda: nc.vector.tensor_scalar(OA[:, h, :], bp[:, slot * 65:(slot + 1) * 65], S_[:, h, n:n + 1], None, ALU.mult), [bp, S_], [OA])
                            else:
                                fw.op("dve", lambda: nc.vector.scalar_tensor_tensor(OA[:, h, :], bp[:, slot * 65:(slot + 1) * 65], S_[:, h, n:n + 1], OA[:, h, :], ALU.mult, ALU.add), [bp, S_, OA], [OA])
                rd = rdn[u]
                OB = OBs[u]
                fw.op("dve", lambda: nc.vector.reciprocal(rd[:, :], OA[:, :, 64]), [OA], [rd])
                fw.op("pool", lambda: nc.gpsimd.tensor_tensor(OB[:, :, :], OA[:, :, 0:64], rd[:, :].unsqueeze(2).to_broadcast([128, 8, 64]), ALU.mult), [OA, rd], [OB])
                fw.dma("pool", O[j * 128:(j + 1) * 128, 0:512], OB[:, :, :].rearrange("p h c -> p (h c)"), reads=[OB], writes=[O])
        fw.barrier()
        with ExitStack() as es:
            NTRI = fw.sb(es, "NTRI", [128, 128], BF16)
            MS = fw.sb(es, "MS", [128, 128], BF16)
            ones = fw.sb(es, "ones", [128, 1], BF16)
            with ExitStack() as es2:
                cst_t = fw.sb(es2, "cst_t", [128, C_END], F32)
                fw.dma("sp", cst_t[:, :], d["consts"][:, :], reads=[d["consts"]], writes=[cst_t])
                fw.op("dve", lambda: nc.vector.tensor_scalar(NTRI[:, :], cst_t[:, C_TRI:C_TRI + 128], 8.0, None, ALU.mult), [cst_t], [NTRI])
                fw.op("dve", lambda: nc.vector.tensor_copy(MS[:, :], cst_t[:, C_SBM:C_SBM + 128]), [cst_t], [MS])
            fw.barrier()
            fw.op("pool", lambda: nc.gpsimd.memset(ones[:, :], 1.0), [], [ones])
            QD = fw.sb(es, "QD", [128, 4, S], BF16)
            KD = fw.sb(es, "KD", [128, 4, S], BF16)
            VD = fw.sb(es, "VD", [128, NT, 512], BF16)
            for c in range(4):
                fw.dma("sp", QD[:, c, :], FM[(6 + c) * 128:(7 + c) * 128, :], reads=[FM], writes=[QD])
                fw.dma("sp", KD[:, c, :], FM[(10 + c) * 128:(11 + c) * 128, :], reads=[FM], writes=[KD])
            fw.dma("sp", VD[:, :, :], tmv[:, :, 128:640], reads=[TM], writes=[VD])
            zps = [fw.ps(es, "zps%d" % i, [128, 512], F32) for i in range(3)]
            ops_ = [fw.ps(es, "ops%d" % i, [128, 512], F32) for i in range(2)]
            E32 = [fw.sb(es, "E32_%d" % i, [128, 512], F32) for i in range(2)]
            SPM = [fw.sb(es, "SPM%d" % i, [128, 512], BF16) for i in range(3)]
            WT = [fw.sb(es, "WT%d" % i, [128, 512], BF16) for i in range(3)]
            Rs = [fw.sb(es, "R%d" % i, [128, 4], F32) for i in range(2)]
            Fs = [fw.sb(es, "F%d" % i, [128, 4], F32) for i in range(3)]
            OAs = [fw.sb(es, "OD%d" % i, [128, 4, 64], F32) for i in range(2)]
            OGs = [fw.sb(es, "OG%d" % i, [128, 4, 512], BF16) for i in range(2)]
            cz = co = cf = ch = 0
            for G in range(8):
                OG = OGs[G % 2]
                for h in range(8):
                    pr = slice(64 * (h % 2), 64 * (h % 2) + 64)
                    pair = h // 2
                    R = Rs[ch % 2]
                    OA = OAs[ch % 2]
                    ch += 1
                    fw.op("pool", lambda: nc.gpsimd.memset(R[:, :], 0.0), [], [R])
                    for kt in range(4 * G + 3, -1, -1):
                        i0 = max(0, kt - 4 * G)
                        c0 = i0 * 128
                        diag = kt >= 4 * G
                        zp = zps[cz % 3]
                        e32 = E32[cz % 2]
                        spm = SPM[cz % 3]
                        wt = WT[cz % 3]
                        cz += 1
                        fw.op("pe", lambda: nc.tensor.matmul(zp[:, c0:512], KD[pr, pair, kt * 128:(kt + 1) * 128], QD[pr, pair, G * 512 + c0:(G + 1) * 512],
                                                             start=True, stop=False, skip_group_check=True), [KD, QD], [zp])
                        fw.op("act", lambda: nc.scalar.activation(e32[:, c0:512], zp[:, c0:512], AF.Exp, scale=0.125), [zp], [e32])
                        fw.op("act", lambda: nc.scalar.activation(spm[:, c0:512], e32[:, c0:512], AF.Ln, bias=1.0), [e32], [spm])
                        if diag:
                            fw.op("pool", lambda: nc.gpsimd.tensor_tensor(spm[:, c0:c0 + 128], spm[:, c0:c0 + 128], MS[:, :], ALU.mult), [spm, MS], [spm])
                        fw.op("pe", lambda: nc.tensor.matmul(zp[:, c0:512], NTRI[:, :], spm[:, c0:512], start=False, stop=True, skip_group_check=True), [NTRI, spm], [zp])
                        fw.op("act", lambda: nc.scalar.activation(wt[:, c0:512], zp[:, c0:512], AF.Exp, scale=0.125), [zp], [wt])
                        if diag:
                            fw.op("pool", lambda: nc.gpsimd.tensor_tensor(wt[:, c0:c0 + 128], wt[:, c0:c0 + 128], MS[:, :], ALU.mult), [wt, MS], [wt])
                        op_ = ops_[co % 2]
                        co += 1
                        for i in range(i0, 4):
                            fw.op("pe", lambda: nc.tensor.matmul(op_[:, i * 65:i * 65 + 64], wt[:, i * 128:(i + 1) * 128], VD[:, kt, h * 64:(h + 1) * 64], start=True, stop=True), [wt, VD], [op_])
                            fw.op("pe", lambda: nc.tensor.matmul(op_[:, i * 65 + 64:i * 65 + 65], spm[:, i * 128:(i + 1) * 128], ones[:, 0:1], start=True, stop=True), [spm, ones], [op_])
                        F = Fs[cf % 3]
                        cf += 1
                        fw.op("act", lambda: nc.scalar.activation(F[:, :], R[:, :], AF.Exp, scale=-1.0), [R], [F])
                        if G == 0 and h == 0 and kt in (3, 0):
                            self.dump("D_E32_%d" % kt, e32, e32[:, :], [128, 512])
                            self.dump("D_SPM_%d" % kt, spm, spm[:, :], [128, 512], BF16)
                            self.dump("D_WT_%d" % kt, wt, wt[:, :], [128, 512], BF16)
                            self.dump("D_F_%d" % kt, F, F[:, :], [128, 4])
                            if ("D_OP_%d" % kt) in self.dbg:
                                dtmp = fw.sb(es, "dtmp%d" % kt, [128, 512], F32)
                                fw.op("dve", lambda: nc.vector.tensor_copy(dtmp[:, 0:260], op_[:, 0:260]), [op_], [dtmp])
                                self.dump("D_OP_%d" % kt, dtmp, dtmp[:, :], [128, 512])
                                dtmp2 = fw.sb(es, "dtmpz%d" % kt, [128, 512], F32)
                                fw.op("dve", lambda: nc.vector.tensor_copy(dtmp2[:, :], zp[:, :]), [zp], [dtmp2])
                                self.dump("D_ZP_%d" % kt, dtmp2, dtmp2[:, :], [128, 512])
                        for i in range(i0, 4):
                            if diag and i == i0:
                                fw.op("dve", lambda: nc.vector.tensor_copy(OA[:, i, :], op_[:, i * 65:i * 65 + 64]), [op_], [OA])
                            else:
                                fw.op("dve", lambda: nc.vector.scalar_tensor_tensor(OA[:, i, :], op_[:, i * 65:i * 65 + 64], F[:, i:i + 1], OA[:, i, :], ALU.mult, ALU.add), [op_, F, OA], [OA])
                        fw.op("dve", lambda: nc.vector.tensor_tensor(R[:, i0:4], R[:, i0:4], op_[:, 0:260].rearrange("p (s c) -> p s c", s=4)[:, i0:4, 64], ALU.add), [R, op_], [R])
                    if G == 0 and h == 0:
                        self.dump("D_OA", OA, OA[:, :, :].rearrange("p i c -> p (i c)"), [128, 256])
                        self.dump("D_R", R, R[:, :], [128, 4])
                    fw.op("pool", lambda: nc.gpsimd.tensor_copy(OG[:, :, h * 64:(h + 1) * 64], OA[:, :, :]), [OA], [OG])
                fw.dma("pool", O.ap()[G * 512:(G + 1) * 512, 512:1024].rearrange("(i p) c -> p i c", p=128), OG[:, :, :], reads=[OG], writes=[O])
        fw.barrier()

    def declare(self):
        specs = {}
        def I(name, shape, dt=F32):
            specs[name] = ("in", shape, dt)
        def Sc(name, shape, dt=BF16, out=False):
            specs[name] = ("out" if out else "scr", shape, dt)
        I("x", [S, D]); I("cT", [128, 8]); I("tabs", [128, T_END]); I("consts", [128, C_END]); I("ebig", [128, 4096])
        I("mod_w", [4 * 1024, 3072]); I("mod_b", [4, 3072]); I("norm_w", [4, 2048])
        I("w_in_ab", [1024, 2072]); I("w_out_ab", [1024, 1024]); I("cmp_w", [32 * 64, 128]); I("cmp_pe", [64, 32]); I("sinks", [1, 8])
        I("w_in_cd", [1024, 2304]); I("w_out_cd", [1024, 1024]); I("ffn_w_in", [2 * 1024, 2 * DFF]); I("ffn_w_out", [2 * DFF, 1024])
        Sc("MODV", [4, 3072], F32); Sc("FM0", [16 * 128, S]); Sc("TM0", [S, 408]); Sc("FM1", [14 * 128, S]); Sc("FMF", [6 * 128, S], F32)
        Sc("TM1", [S, 640]); Sc("VCD", [256, 256]); Sc("KCTD", [128, 512]); Sc("SELD", [S, 128]); Sc("SEL1", [S, 128], F32)
        Sc("O0", [S, D]); Sc("O1", [S, D]); Sc("X1", [S, D], F32); Sc("X2", [S, D], F32); Sc("X3", [S, D], F32); Sc("Y", [S, D], F32, out=True)
        prog = self

        class Lazy(dict):
            def __missing__(self, name):
                kind, shape, dt = specs[name]
                if kind == "in":
                    prog.din(name, shape, dt)
                else:
                    prog.dscr(name, shape, dt, out=(kind == "out"))
                return dict.__getitem__(self, name)

        self.dram = Lazy()
        self.in_names = []
        self.out_names = []


def build(phases, dbg=(), xin_override=None, as_input=()):
    p = Prog(dbg, as_input)
    p.declare()
    d = p.dram
    nc = p.nc
    with ExitStack() as es:
        p.fw = FW(nc, es)
        p.phase_setup(es)
        for ph in phases:
            if ph == "modvec":
                p.phase_modvec()
            elif ph == "proj0":
                p.phase_proj(0, d["x"])
            elif ph == "proj1":
                p.phase_proj(1, d["X2"] if xin_override is None else d[xin_override])
            elif ph == "mix0":
                p.phase_mix0()
            elif ph == "mix1":
                p.phase_mix1()
            elif ph == "out0":
                p.phase_outproj(0, d["x"], d["X1"])
            elif ph == "out1":
                p.phase_outproj(1, d["X2"] if xin_override is None else d[xin_override], d["X3"])
            elif ph == "ffn0":
                p.phase_ffn(0, d["X1"] if xin_override is None else d[xin_override], d["X2"])
            elif ph == "ffn1":
                p.phase_ffn(1, d["X3"] if xin_override is None else d[xin_override], d["Y"])
            else:
                raise ValueError(ph)
        p.fw.barrier()
    return p


ALL_PHASES = ["modvec", "proj0", "mix0", "out0", "ffn0", "proj1", "mix1", "out1", "ffn1"]


def host_inputs(inputs):
    f = lambda a: np.ascontiguousarray(np.asarray(a, dtype=np.float32))
    rel = f(inputs["rel_table"])
    tabs, consts, ebig = _host_tables(rel)
    shared = {
        "tabs": tabs, "consts": consts, "ebig": ebig,
        "mod_w": f(inputs["mod_w"]).reshape(4 * 1024, 3072),
        "mod_b": f(inputs["mod_b"]).reshape(4, 3072),
        "norm_w": f(inputs["norm_w"]).reshape(4, 2048),
        "w_in_ab": f(inputs["w_in_ab"])[0],
        "w_out_ab": f(inputs["w_out_ab"])[0],
        "cmp_w": np.ascontiguousarray(np.concatenate([f(inputs["nsa_cmp_wk"])[0].reshape(2048, 64),
                                                      f(inputs["nsa_cmp_wv"])[0].reshape(2048, 64)], axis=1)),
        "cmp_pe": np.ascontiguousarray(f(inputs["nsa_cmp_pe"])[0].T),
        "sinks": f(inputs["swa_sinks"]).reshape(1, 8),
        "w_in_cd": f(inputs["w_in_cd"])[0],
        "w_out_cd": f(inputs["w_out_cd"])[0],
        "ffn_w_in": f(inputs["ffn_w_in"]).reshape(2 * 1024, 2 * DFF),
        "ffn_w_out": f(inputs["ffn_w_out"]).reshape(2 * DFF, 1024),
    }
    x = f(inputs["x"])
    c = f(inputs["c"])
    maps = []
    for b in range(x.shape[0]):
        m = dict(shared)
        m["x"] = x[b]
        m["cT"] = np.ascontiguousarray(c[b].reshape(8, 128).T)
        maps.append(m)
    return maps


LAUNCHES = [
    (list(ALL_PHASES), set(), set()),
]


def kernel(**inputs):
    maps = host_inputs(inputs)
    n = len(maps)
    carry = [dict() for _ in range(n)]
    res = None
    for phases, outs, as_in in LAUNCHES:
        p = build(phases, dbg=outs, as_input=as_in)
        in_maps = []
        for c in range(n):
            m = {}
            for nm in p.in_names:
                m[nm] = carry[c][nm] if nm in carry[c] else maps[c][nm]
            in_maps.append(m)
        res = run_bass_kernel_spmd(p.nc, in_maps, core_ids=list(range(n)))
        for c in range(n):
            for nm in p.out_names:
                carry[c][nm] = np.ascontiguousarray(np.asarray(res.results[c][nm]))
    return np.stack([np.asarray(carry[c]["Y"], dtype=np.float32) for c in range(n)], axis=0)
```

```python
import math
import os
from contextlib import ExitStack

import numpy as np
import concourse.bass as bass
import concourse.mybir as mybir
from concourse.bass_utils import run_bass_kernel_spmd

F32 = mybir.dt.float32
BF16 = mybir.dt.bfloat16
AF = mybir.ActivationFunctionType
ALU = mybir.AluOpType
AX = mybir.AxisListType

S = 4096
D = 1024
NT = S // 128
DFF = 2816
NFC = DFF // 128
NEG = -30000.0
EPS = 1e-6
PERM = [0, 2, 1, 3]


class Buf:
    def __init__(self, t, name=""):
        self.t = t
        self.name = name
        self.w = {}
        self.r = {}

    def __getitem__(self, idx):
        return self.t[idx]

    def ap(self):
        return self.t.ap()


class FW:
    def __init__(self, nc, es, n_dma_slots=32):
        self.nc = nc
        self.es = es
        self.eng = {"pe": nc.tensor, "act": nc.scalar, "dve": nc.vector, "pool": nc.gpsimd, "sp": nc.sync}
        self.sem = {}
        self.cnt = {}
        for k in self.eng:
            self.sem[k] = es.enter_context(nc.semaphore("s_" + k))
            self.cnt[k] = 0
        self.nslots = n_dma_slots
        for i in range(n_dma_slots):
            k = "d%d" % i
            self.sem[k] = es.enter_context(nc.semaphore("s_" + k))
            self.cnt[k] = 0
        self.next_slot = 0
        self.seen = {e: {} for e in self.eng}
        self.n_ops = 0

    def _uniq(self, name):
        self.n_names = getattr(self, "n_names", 0) + 1
        return "%s_u%d" % (name, self.n_names)

    def sb(self, es, name, shape, dt):
        name = self._uniq(name)
        return Buf(es.enter_context(self.nc.sbuf_tensor(name, shape, dt)), name)

    def ps(self, es, name, shape, dt=F32):
        name = self._uniq(name)
        return Buf(es.enter_context(self.nc.psum_tensor(name, shape, dt)), name)

    def view(self, b, name=""):
        return Buf(b.t, name or b.name)

    def _wait(self, e, toks):
        for k, v in toks.items():
            if v <= 0 or self.seen[e].get(k, 0) >= v:
                continue
            if e == "pe" and k == "pe" and getattr(self, "pe_pipeline", False):
                continue
            self.eng[e].wait_ge(self.sem[k], v)
            self.seen[e][k] = v

    @staticmethod
    def _merge(dst, src):
        for k, v in src.items():
            if v > dst.get(k, 0):
                dst[k] = v

    def _deps(self, reads, writes):
        toks = {}
        for b in reads:
            self._merge(toks, b.w)
        for b in writes:
            self._merge(toks, b.w)
            self._merge(toks, b.r)
        return toks

    def _record(self, key, val, reads, writes):
        for b in writes:
            b.r = {}
            if val > b.w.get(key, 0):
                b.w[key] = val
        for b in reads:
            if val > b.r.get(key, 0):
                b.r[key] = val

    def op(self, e, fn, reads=(), writes=(), kind="mm"):
        if e == "pe":
            if kind != getattr(self, "pe_kind", "mm") and self.cnt["pe"] > self.seen["pe"].get("pe", 0):
                self.eng["pe"].wait_ge(self.sem["pe"], self.cnt["pe"])
                self.seen["pe"]["pe"] = self.cnt["pe"]
            self.pe_kind = kind
        self._wait(e, self._deps(reads, writes))
        ins = fn()
        self.cnt[e] += 1
        ins.then_inc(self.sem[e], 1)
        self._record(e, self.cnt[e], reads, writes)
        self.n_ops += 1

    def dma(self, q, out, in_, reads=(), writes=()):
        s = "d%d" % self.next_slot
        self.next_slot = (self.next_slot + 1) % self.nslots
        toks = self._deps(reads, writes)
        self._merge(toks, {s: self.cnt[s]})
        self._wait(q, toks)
        ins = self.eng[q].dma_start(out=out, in_=in_)
        self.cnt[s] += 16
        ins.then_inc(self.sem[s], 16)
        self._record(s, self.cnt[s], reads, writes)
        self.n_ops += 1

    def barrier(self):
        toks = dict(self.cnt)
        for e in self.eng:
            self._wait(e, toks)


def _rel_bucket(dist):
    n = np.maximum(dist, 0)
    nf = np.maximum(n, 1).astype(np.float32)
    large = 16 + (np.log(nf / np.float32(16)) / np.float32(math.log(128 / 16)) * np.float32(16)).astype(np.int32)
    large = np.minimum(large, 31)
    return np.where(n < 16, n, large).astype(np.int64)


def _host_tables(rel_table):
    kk = np.arange(128)[:, None]
    qq = np.arange(128)[None, :]
    idx0 = _rel_bucket(qq - kk)
    idx1 = _rel_bucket(128 + qq - kk)
    cols = [4 * g + PERM[s] for g in range(2) for s in range(4)]
    cols16 = cols + [8 + c for c in cols]
    toe0 = rel_table[idx0][:, :, cols16].transpose(0, 2, 1)
    toe1 = rel_table[idx1][:, :, cols16].transpose(0, 2, 1)
    b31 = np.broadcast_to(rel_table[31][cols16][None, :], (128, 16))
    def tc(ms):
        m = np.asarray(ms)[:, None]
        d = qq - 16 * m - 31
        t = rel_table[_rel_bucket(d)][:, :, cols[:8]].transpose(0, 2, 1)
        mk = np.where(d >= 0, 0.0, NEG).astype(np.float32)
        full_t = np.zeros((128, 8, 128), np.float32)
        full_m = np.full((128, 128), 0.0, np.float32)
        full_t[: len(ms)] = t
        full_m[: len(ms)] = mk
        return full_t, full_m
    tcg, mcg = tc(range(-9, 7))
    tc0, mc0 = tc(range(0, 7))
    tc1, mc1 = tc(range(-8, 7))
    mask0 = np.where(qq >= kk, 0.0, NEG).astype(np.float32)
    masklt = np.where(qq < kk, 0.0, NEG).astype(np.float32)
    ident = np.eye(128, dtype=np.float32)
    tri = np.where(kk >= qq, -1.0, 0.0).astype(np.float32)
    q512 = np.arange(512)[None, :]
    sbm = np.stack([(128 * i + kk < q512).astype(np.float32) for i in range(4)], axis=1)
    n = np.arange(255)[:, None]
    s = np.arange(64)[None, :]
    ov = np.maximum(np.minimum(16 * n + 32, 64 * s + 64) - np.maximum(16 * n, 64 * s), 0).astype(np.float32) / 32.0
    ovw = np.zeros((128, 2, 64), np.float32)
    ovw[:, 0] = ov[:128]
    ovw[:127, 1] = ov[128:]
    ebig = (np.arange(64)[:, None] == (np.arange(4096)[None, :] // 64)).astype(np.float32)
    ebig128 = np.zeros((128, 4096), np.float32)
    ebig128[:64] = ebig
    tabs = np.concatenate([
        toe0.reshape(128, -1), toe1.reshape(128, -1), b31,
        tcg.reshape(128, -1), tc0.reshape(128, -1), tc1.reshape(128, -1)], axis=1).astype(np.float32)
    consts = np.concatenate([
        mask0, masklt, mcg, mc0, mc1, ident, tri, sbm.reshape(128, -1), ovw.reshape(128, -1)], axis=1).astype(np.float32)
    return np.ascontiguousarray(tabs), np.ascontiguousarray(consts), np.ascontiguousarray(ebig128)


T_TOE0 = 0
T_TOE1 = T_TOE0 + 16 * 128
T_B31 = T_TOE1 + 16 * 128
T_TCG = T_B31 + 16
T_TC0 = T_TCG + 8 * 128
T_TC1 = T_TC0 + 8 * 128
T_END = T_TC1 + 8 * 128
C_MASK0 = 0
C_MASKLT = 128
C_MCG = 256
C_MC0 = 384
C_MC1 = 512
C_IDENT = 640
C_TRI = 768
C_SBM = 896
C_OVW = C_SBM + 2048
C_END = C_OVW + 128


class Prog:
    def __init__(self, dbg=(), as_input=()):
        self.dbg = set(dbg)
        self.as_input = set(as_input)
        self.nc = bass.Bass("TRN2", target_bir_lowering=False)
        self.dram = {}

    def din(self, name, shape, dt=F32):
        t = self.nc.dram_tensor(name, list(shape), dt, kind="ExternalInput")
        self.dram[name] = Buf(t, name)
        self.in_names.append(name)
        return self.dram[name]

    def dscr(self, name, shape, dt=BF16, out=False):
        kind = "ExternalOutput" if (out or name in self.dbg) else "Internal"
        if name in self.as_input:
            kind = "ExternalInput"
        t = self.nc.dram_tensor(name, list(shape), dt, kind=kind)
        self.dram[name] = Buf(t, name)
        if kind == "ExternalInput":
            self.in_names.append(name)
        if kind == "ExternalOutput":
            self.out_names.append(name)
        return self.dram[name]

    def dump(self, name, buf, ap, shape, dt=F32):
        if name not in self.dbg:
            return
        fw, nc = self.fw, self.nc
        if not dict.__contains__(self.dram, name):
            self.dscr(name, shape, dt)
        fw.dma("pool", self.dram[name].ap(), ap, reads=[buf], writes=[self.dram[name]])

    def load_w(self, es, dst, dst_c0, src, r0, c0, ncols, nk, tag, dup=False):
        fw, nc = self.fw, self.nc
        srcv = src.ap()[r0:r0 + nk * 128, :].rearrange("(k p) c -> p k c", p=128)
        CW = 2048 // nk if nk <= 8 else 64
        CW = max(64, min(512, CW))
        for cc in range(0, ncols, CW):
            w = min(CW, ncols - cc)
            st = self.wstage[self.wstage_i % len(self.wstage)]
            self.wstage_i += 1
            stv = st[:, 0:nk * w].rearrange("p (k c) -> p k c", k=nk)
            fw.dma("sp", stv, srcv[:, :, c0 + cc:c0 + cc + w], reads=[src], writes=[st])
            e = ["pool", "dve"][self.wstage_i % 2]
            eng = fw.eng[e]
            fw.op(e, lambda: eng.tensor_copy(dst[:, 0:nk, dst_c0 + cc:dst_c0 + cc + w], stv), [st], [dst])
            if dup:
                e2 = ["dve", "pool"][self.wstage_i % 2]
                eng2 = fw.eng[e2]
                fw.op(e2, lambda: eng2.tensor_copy(dst[:, 0:nk, dst_c0 + 64 + cc:dst_c0 + 64 + cc + w], stv), [st], [dst])

    def alloc_wstage(self, es):
        self.wstage = [self.fw.sb(es, "wst%d" % i, [128, 2048], F32) for i in range(2)]
        self.wstage_i = 0

    def load_bcast(self, dst, src_buf, src_row_ap):
        self.fw.dma("sp", dst[:, :], src_row_ap.partition_broadcast(128), reads=[src_buf], writes=[dst])

    def rstd_from_ss(self, ss, tmp, rstd):
        fw, nc = self.fw, self.nc
        fw.op("act", lambda: nc.scalar.activation(tmp[:, :], ss[:, :], AF.Ln, scale=1.0 / D, bias=self.eps_t[:, 0:1]), [ss, self.eps_t], [tmp])
        fw.op("act", lambda: nc.scalar.activation(rstd[:, :], tmp[:, :], AF.Exp, scale=-0.5), [tmp], [rstd])

    def norm_mod_T(self, xt, A, Bv, hT_dst, hT_buf, col0, tmps, tp_ps, n_tok_cols=128):
        fw, nc = self.fw, self.nc
        junk, ss, s1, rstd, h32, hb = tmps
        fw.op("act", lambda: nc.scalar.activation(junk[:, :], xt[:, :], AF.Square, accum_out=ss[:, :]), [xt], [junk, ss])
        self.rstd_from_ss(ss, s1, rstd)
        fw.op("dve", lambda: nc.vector.scalar_tensor_tensor(h32[:, :], xt[:, :], rstd[:, 0:1], A[:, :], ALU.mult, ALU.mult), [xt, rstd, A], [h32])
        fw.op("pool", lambda: nc.gpsimd.tensor_tensor(hb[:, :], h32[:, :], Bv[:, :], ALU.add), [h32, Bv], [hb])
        for k in range(8):
            fw.op("pe", lambda: nc.tensor.transpose(tp_ps[:, k * 128:(k + 1) * 128], hb[:, k * 128:(k + 1) * 128], self.identb[:, :]), [hb, self.identb], [tp_ps], kind="tr")
        fw.op("act", lambda: nc.scalar.copy(hT_dst[:, :, col0:col0 + 128], tp_ps[:, :].rearrange("p (k c) -> p k c", k=8)), [tp_ps], [hT_buf])

    def post_norm_residual(self, yps, xt, Gv, xn, tmps):
        fw, nc = self.fw, self.nc
        junk, ss2, ss, s1, rstd, t32 = tmps
        for h in range(2):
            fw.op("act", lambda: nc.scalar.activation(junk[:, h * 512:(h + 1) * 512], yps[h][:, :], AF.Square, accum_out=ss2[:, h:h + 1]), [yps[h]], [junk, ss2])
        fw.op("dve", lambda: nc.vector.tensor_tensor(ss[:, :], ss2[:, 0:1], ss2[:, 1:2], ALU.add), [ss2], [ss])
        self.rstd_from_ss(ss, s1, rstd)
        for h in range(2):
            sl = slice(h * 512, (h + 1) * 512)
            fw.op("dve", lambda: nc.vector.scalar_tensor_tensor(t32[:, sl], yps[h][:, :], rstd[:, 0:1], Gv[:, sl], ALU.mult, ALU.mult), [yps[h], rstd, Gv], [t32])
        fw.op("pool", lambda: nc.gpsimd.tensor_tensor(xn[:, :], t32[:, :], xt[:, :], ALU.add), [t32, xt], [xn])

    def phase_setup(self, es):
        fw, nc = self.fw, self.nc
        d = self.dram
        self.eps_t = fw.sb(es, "eps_t", [128, 1], F32)
        fw.op("pool", lambda: nc.gpsimd.memset(self.eps_t[:, :], EPS), [], [self.eps_t])
        self.identb = fw.sb(es, "identb", [128, 128], BF16)
        self.identf = fw.sb(es, "identf", [128, 128], F32)
        fw.dma("sp", self.identf[:, :], d["consts"][:, C_IDENT:C_IDENT + 128], reads=[d["consts"]], writes=[self.identf])
        fw.op("dve", lambda: nc.vector.tensor_copy(self.identb[:, :], self.identf[:, :]), [self.identf], [self.identb])

    def phase_modvec(self):
        fw, nc = self.fw, self.nc
        d = self.dram
        with ExitStack() as es:
            cT = fw.sb(es, "cT", [128, 8], F32)
            fw.dma("sp", cT[:, :], d["cT"][:, :], reads=[d["cT"]], writes=[cT])
            wst = [fw.sb(es, "mw%d" % i, [128, 8, 512], F32) for i in range(2)]
            mps = [fw.ps(es, "mps%d" % i, [128, 512], F32) for i in range(2)]
            mv = fw.sb(es, "mv", [128, 3072], F32)
            mb = fw.sb(es, "mb", [128, 3072], F32)
            nw = fw.sb(es, "nw", [128, 2048], F32)
            res = fw.sb(es, "mres", [128, 3072], F32)
            i = 0
            for ls in range(4):
                l, s = ls // 2, ls % 2
                self.load_bcast(mb, d["mod_b"], d["mod_b"][ls:ls + 1, :])
                self.load_bcast(nw, d["norm_w"], d["norm_w"][ls:ls + 1, :])
                wv = d["mod_w"].ap()[ls * 1024:(ls + 1) * 1024, :].rearrange("(k p) c -> p k c", p=128)
                for nt in range(6):
                    st = wst[i % 2]
                    ps = mps[i % 2]
                    i += 1
                    fw.dma("sp", st[:, :, :], wv[:, :, nt * 512:(nt + 1) * 512], reads=[d["mod_w"]], writes=[st])
                    for k in range(8):
                        fw.op("pe", lambda: nc.tensor.matmul(ps[:, :], cT[:, k:k + 1].to_broadcast([128, 128]), st[:, k, :], start=(k == 0), stop=(k == 7)), [cT, st], [ps], kind="f32")
                    fw.op("dve", lambda: nc.vector.tensor_tensor(mv[:, nt * 512:(nt + 1) * 512], ps[:, :], mb[:, nt * 512:(nt + 1) * 512], ALU.add), [ps, mb], [mv])
                fw.op("dve", lambda: nc.vector.scalar_tensor_tensor(res[:, 0:1024], mv[:, 1024:2048], 1.0, nw[:, 0:1024], ALU.add, ALU.mult), [mv, nw], [res])
                fw.op("pool", lambda: nc.gpsimd.tensor_copy(res[:, 1024:2048], mv[:, 0:1024]), [mv], [res])
                fw.op("dve", lambda: nc.vector.tensor_tensor(res[:, 2048:3072], mv[:, 2048:3072], nw[:, 1024:2048], ALU.mult), [mv, nw], [res])
                fw.dma("pool", d["MODV"][ls:ls + 1, :], res[0:1, :], reads=[res], writes=[d["MODV"]])
        fw.barrier()

    def load_modv(self, es, ls, which):
        d = self.dram
        out = []
        for i, nm in enumerate("ABG"):
            if nm in which:
                t = self.fw.sb(es, "mod%s" % nm, [128, 1024], F32)
                self.load_bcast(t, d["MODV"], d["MODV"][ls:ls + 1, i * 1024:(i + 1) * 1024])
                out.append(t)
        return out

    def phase_proj(self, l, xin):
        fw, nc = self.fw, self.nc
        d = self.dram
        if l == 0:
            src = d["w_in_ab"]
            fm = [(0, 128, False), (128, 128, False), (256, 128, False), (384, 128, False),
                  (1304, 128, False), (1432, 128, False), (1560, 128, False), (1688, 128, False),
                  (512, 128, False), (640, 128, False),
                  (768, 64, True), (832, 64, True), (1024, 64, True), (1088, 64, True),
                  (1816, 64, True), (1880, 64, True)]
            tm = [(896, 128), (1152, 128), (1944, 128), (1280, 24)]
            FM, TM = d["FM0"], d["TM0"]
            f32_chunks = {}
        else:
            src = d["w_in_cd"]
            fm = [(0, 128, False), (128, 128, False), (256, 128, False), (384, 128, False),
                  (512, 64, True), (576, 64, True),
                  (768, 128, False), (896, 128, False), (1024, 128, False), (1152, 128, False),
                  (1280, 128, False), (1408, 128, False), (1536, 128, False), (1664, 128, False)]
            tm = [(640, 128), (1792, 512)]
            FM, TM = d["FM1"], d["TM1"]
            f32_chunks = {0: 0, 1: 1, 2: 2, 3: 3, 4: 4, 5: 5}
        nfm = len(fm)
        ntm = sum(c for _, c in tm)
        with ExitStack() as es:
            self.alloc_wstage(es)
            Wfm = fw.sb(es, "Wfm", [128, 8, nfm * 128], BF16)
            Wtm = fw.sb(es, "Wtm", [128, 8, ntm], BF16)
            for ci, (c0, ncl, dup) in enumerate(fm):
                self.load_w(es, Wfm, ci * 128, src, 0, c0, ncl, 8, "fm", dup=dup)
            o = 0
            for (c0, ncl) in tm:
                self.load_w(es, Wtm, o, src, 0, c0, ncl, 8, "tm")
                o += ncl
            A, Bv = self.load_modv(es, 2 * l, "AB")
            xts = [fw.sb(es, "xt%d" % i, [128, 1024], F32) for i in range(2)]
            junk = fw.sb(es, "junk", [128, 1024], F32)
            h32 = fw.sb(es, "h32", [128, 1024], F32)
            hbs = [fw.sb(es, "hb%d" % i, [128, 1024], BF16) for i in range(2)]
            smalls = [[fw.sb(es, "sm%d_%d" % (i, j), [128, 1], F32) for j in range(3)] for i in range(2)]
            hTs = [fw.sb(es, "hT%d" % i, [128, 8, 512], BF16) for i in range(2)]
            tps = [fw.ps(es, "tp%d" % i, [128, 1024], BF16) for i in range(2)]
            fps = [fw.ps(es, "fps%d" % i, [128, 512], F32) for i in range(3)]
            tmps_ = [fw.ps(es, "tmps%d" % i, [128, 512], F32) for i in range(2)]
            stg = [fw.sb(es, "stg%d" % i, [128, 512], BF16) for i in range(4)]
            stgf = [fw.sb(es, "stgf%d" % i, [128, 512], F32) for i in range(2)]
            stt = [fw.sb(es, "stt%d" % i, [128, ntm], BF16) for i in range(2)]
            ti = 0
            si = 0
            for st_ in range(8):
                hT = hTs[st_ % 2]
                for tt in range(4):
                    t = st_ * 4 + tt
                    xt = xts[t % 2]
                    fw.dma("sp", xt[:, :], xin[t * 128:(t + 1) * 128, :], reads=[xin], writes=[xt])
                    sm = smalls[t % 2]
                    self.norm_mod_T(xt, A, Bv, hT, hT, tt * 128, (junk, sm[0], sm[1], sm[2], h32, hbs[t % 2]), tps[t % 2])
                for ci in range(nfm):
                    ps = fps[ci % 3]
                    for k in range(8):
                        fw.op("pe", lambda: nc.tensor.matmul(ps[:, :], Wfm[:, k, ci * 128:(ci + 1) * 128], hT[:, k, :], start=(k == 0), stop=(k == 7)), [Wfm, hT], [ps])
                    sg = stg[si % 4]
                    si += 1
                    if ci in f32_chunks:
                        sf = stgf[ci % 2]
                        fw.op("dve", lambda: nc.vector.tensor_copy(sf[:, :], ps[:, :]), [ps], [sf])
                        fw.op("act", lambda: nc.scalar.copy(sg[:, :], sf[:, :]), [sf], [sg])
                        fi = f32_chunks[ci]
                        fw.dma("sp", d["FMF"][fi * 128:(fi + 1) * 128, st_ * 512:(st_ + 1) * 512], sf[:, :], reads=[sf], writes=[d["FMF"]])
                    elif ci % 2 == 0:
                        fw.op("act", lambda: nc.scalar.copy(sg[:, :], ps[:, :]), [ps], [sg])
                    else:
                        fw.op("dve", lambda: nc.vector.tensor_copy(sg[:, :], ps[:, :]), [ps], [sg])
                    fw.dma("pool", FM[ci * 128:(ci + 1) * 128, st_ * 512:(st_ + 1) * 512], sg[:, :], reads=[sg], writes=[FM])
                for tt in range(4):
                    t = st_ * 4 + tt
                    so = stt[t % 2]
                    for c0 in range(0, ntm, 512):
                        w = min(512, ntm - c0)
                        ps = tmps_[ti % 2]
                        ti += 1
                        for k in range(8):
                            fw.op("pe", lambda: nc.tensor.matmul(ps[:, 0:w], hT[:, k, tt * 128:(tt + 1) * 128], Wtm[:, k, c0:c0 + w], start=(k == 0), stop=(k == 7)), [hT, Wtm], [ps])
                        fw.op("dve", lambda: nc.vector.tensor_copy(so[:, c0:c0 + w], ps[:, 0:w]), [ps], [so])
                    fw.dma("pool", TM[t * 128:(t + 1) * 128, :], so[:, :], reads=[so], writes=[TM])
        fw.barrier()

    def phase_outproj(self, l, xin, xout):
        fw, nc = self.fw, self.nc
        d = self.dram
        src = d["w_out_ab"] if l == 0 else d["w_out_cd"]
        O = d["O%d" % l]
        with ExitStack() as es:
            self.alloc_wstage(es)
            Wo = fw.sb(es, "Wo", [128, 8, 1024], BF16)
            for c in range(0, 1024, 256):
                self.load_w(es, Wo, c, src, 0, c, 256, 8, "wo")
            (Gv,) = self.load_modv(es, 2 * l, "G")
            ots = [fw.sb(es, "ot%d" % i, [128, 1024], BF16) for i in range(2)]
            oTs = [fw.sb(es, "oT%d" % i, [128, 8, 128], BF16) for i in range(2)]
            xts = [fw.sb(es, "xt%d" % i, [128, 1024], F32) for i in range(2)]
            xns = [fw.sb(es, "xn%d" % i, [128, 1024], F32) for i in range(2)]
            junk = fw.sb(es, "junk", [128, 1024], F32)
            t32 = fw.sb(es, "t32", [128, 1024], F32)
            smalls = [[fw.sb(es, "sm%d_%d" % (i, j), [128, 2 if j == 0 else 1], F32) for j in range(4)] for i in range(2)]
            tps = [fw.ps(es, "tp%d" % i, [128, 1024], BF16) for i in range(2)]
            yps = [[fw.ps(es, "y%d_%d" % (i, h), [128, 512], F32) for h in range(2)] for i in range(2)]
            for t in range(NT):
                ot, oT, xt, xn = ots[t % 2], oTs[t % 2], xts[t % 2], xns[t % 2]
                fw.dma("sp", ot[:, :], O[t * 128:(t + 1) * 128, :], reads=[O], writes=[ot])
                fw.dma("sp", xt[:, :], xin[t * 128:(t + 1) * 128, :], reads=[xin], writes=[xt])
                tp = tps[t % 2]
                for k in range(8):
                    fw.op("pe", lambda: nc.tensor.transpose(tp[:, k * 128:(k + 1) * 128], ot[:, k * 128:(k + 1) * 128], self.identb[:, :]), [ot, self.identb], [tp], kind="tr")
                fw.op("dve", lambda: nc.vector.tensor_copy(oT[:, :, :], tp[:, :].rearrange("p (k c) -> p k c", k=8)), [tp], [oT])
                yp = yps[t % 2]
                for h in range(2):
                    for k in range(8):
                        fw.op("pe", lambda: nc.tensor.matmul(yp[h][:, :], oT[:, k, :], Wo[:, k, h * 512:(h + 1) * 512], start=(k == 0), stop=(k == 7)), [oT, Wo], [yp[h]])
                sm = smalls[t % 2]
                self.post_norm_residual(yp, xt, Gv, xn, (junk, sm[0], sm[1], sm[2], sm[3], t32))
                fw.dma("pool", xout[t * 128:(t + 1) * 128, :], xn[:, :], reads=[xn], writes=[xout])
        fw.barrier()

    def phase_ffn(self, l, xin, xout):
        fw, nc = self.fw, self.nc
        d = self.dram
        win, wout = d["ffn_w_in"], d["ffn_w_out"]
        with ExitStack() as es:
            self.alloc_wstage(es)
            Wi = fw.sb(es, "Wi", [128, 8, 2 * DFF], BF16)
            Wo = fw.sb(es, "Wo2", [128, NFC, 1024], BF16)
            A, Bv, Gv = self.load_modv(es, 2 * l + 1, "ABG")
            xts = [fw.sb(es, "xt%d" % i, [128, 1024], F32) for i in range(2)]
            xns = [fw.sb(es, "xn%d" % i, [128, 1024], F32) for i in range(2)]
            junk = fw.sb(es, "junk", [128, 1024], F32)
            h32 = fw.sb(es, "h32", [128, 1024], F32)
            hb = fw.sb(es, "hb", [128, 1024], BF16)
            smalls = [[fw.sb(es, "sm%d_%d" % (i, j), [128, 2 if j == 3 else 1], F32) for j in range(7)] for i in range(2)]
            hTs = [fw.sb(es, "hT%d" % i, [128, 8, 256], BF16) for i in range(2)]
            aTs = [fw.sb(es, "aT%d" % i, [128, 256], BF16) for i in range(3)]
            sgs = [fw.sb(es, "sg%d" % i, [128, 256], F32) for i in range(2)]
            tp = fw.ps(es, "tp", [128, 1024], BF16)
            gu = [fw.ps(es, "gu%d" % i, [128, 512], F32) for i in range(2)]
            yps = [[fw.ps(es, "y%d_%d" % (i, h), [128, 512], F32) for h in range(2)] for i in range(2)]
            for c in range(0, 2 * DFF, 256):
                self.load_w(es, Wi, c, win, l * 1024, c, 256, 8, "wi")
            for k0 in range(0, NFC, 2):
                srcv = wout.ap()[l * DFF + k0 * 128: l * DFF + (k0 + 2) * 128, :].rearrange("(k p) c -> p k c", p=128)
                st = self.wstage[self.wstage_i % 2]
                self.wstage_i += 1
                stv = st[:, :].rearrange("p (k c) -> p k c", k=2)
                fw.dma("sp", stv, srcv, reads=[wout], writes=[st])
                e = ["pool", "dve"][self.wstage_i % 2]
                eng = fw.eng[e]
                fw.op(e, lambda: eng.tensor_copy(Wo[:, k0:k0 + 2, :], stv), [st], [Wo])
            ai = 0
            for st_ in range(16):
                hT = hTs[st_ % 2]
                for tt in range(2):
                    t = st_ * 2 + tt
                    xt = xts[tt]
                    fw.dma("sp", xt[:, :], xin[t * 128:(t + 1) * 128, :], reads=[xin], writes=[xt])
                    sm = smalls[tt]
                    self.norm_mod_T(xt, A, Bv, hT, hT, tt * 128, (junk, sm[0], sm[1], sm[2], h32, hb), tp)
                for f in range(NFC):
                    g = gu[f % 2]
                    for half, c0 in ((0, f * 128), (1, DFF + f * 128)):
                        for k in range(8):
                            fw.op("pe", lambda: nc.tensor.matmul(g[:, half * 256:(half + 1) * 256], Wi[:, k, c0:c0 + 128], hT[:, k, :], start=(k == 0), stop=(k == 7)), [Wi, hT], [g])
                    sg = sgs[f % 2]
                    aT = aTs[ai % 3]
                    ai += 1
                    fw.op("act", lambda: nc.scalar.activation(sg[:, :], g[:, 0:256], AF.Silu), [g], [sg])
                    fw.op("dve", lambda: nc.vector.tensor_tensor(aT[:, :], sg[:, :], g[:, 256:512], ALU.mult), [sg, g], [aT])
                    for tt in range(2):
                        for h in range(2):
                            fw.op("pe", lambda: nc.tensor.matmul(yps[tt][h][:, :], aT[:, tt * 128:(tt + 1) * 128], Wo[:, f, h * 512:(h + 1) * 512], start=(f == 0), stop=(f == NFC - 1)), [aT, Wo], [yps[tt][h]])
                for tt in range(2):
                    t = st_ * 2 + tt
                    sm = smalls[tt]
                    xn = xns[tt]
                    self.post_norm_residual(yps[tt], xts[tt], Gv, xn, (junk, sm[3], sm[4], sm[5], sm[6], h32))
                    fw.dma("pool", xout[t * 128:(t + 1) * 128, :], xn[:, :], reads=[xn], writes=[xout])
        fw.barrier()

    def build_bias_tables(self, es, tabs_t, cst_t, base_pos, use_swa_mask):
        fw, nc = self.fw, self.nc
        T0 = fw.sb(es, "T0", [128, 8, 128], F32)
        T1 = fw.sb(es, "T1", [128, 8, 128], F32)
        for pos in range(8):
            gp = base_pos + pos
            b31 = tabs_t[:, T_B31 + gp:T_B31 + gp + 1]
            toe0 = tabs_t[:, T_TOE0 + gp * 128:T_TOE0 + (gp + 1) * 128]
            toe1 = tabs_t[:, T_TOE1 + gp * 128:T_TOE1 + (gp + 1) * 128]
            fw.op("dve", lambda: nc.vector.scalar_tensor_tensor(T0[:, pos, :], toe0, b31, cst_t[:, C_MASK0:C_MASK0 + 128], ALU.subtract, ALU.add), [tabs_t, cst_t], [T0])
            if use_swa_mask:
                fw.op("dve", lambda: nc.vector.scalar_tensor_tensor(T1[:, pos, :], toe1, b31, cst_t[:, C_MASKLT:C_MASKLT + 128], ALU.subtract, ALU.add), [tabs_t, cst_t], [T1])
            else:
                fw.op("dve", lambda: nc.vector.tensor_scalar(T1[:, pos, :], toe1, b31, None, ALU.subtract), [tabs_t], [T1])
        return T0, T1

    def load_tabs(self, es):
        fw = self.fw
        d = self.dram
        tabs_t = fw.sb(es, "tabs_t", [128, T_END], F32)
        cst_t = fw.sb(es, "cst_t", [128, C_END], F32)
        fw.dma("sp", tabs_t[:, :], d["tabs"][:, :], reads=[d["tabs"]], writes=[tabs_t])
        fw.dma("sp", cst_t[:, :], d["consts"][:, :], reads=[d["consts"]], writes=[cst_t])
        return tabs_t, cst_t

    def score_tile(self, ps, KT, g, kt_lo, kt_n, QT, j, mask=None):
        fw, nc = self.fw, self.nc
        first = True
        if mask is not None:
            E, NEGT = mask
            fw.op("pe", lambda: nc.tensor.matmul(ps[0:kt_n, 0:512], E, NEGT[0:64, :, :], start=True, stop=False, skip_group_check=True), [NEGT, self.ebig_t], [ps])
            first = False
        for half in range(2):
            pr = slice(64 * half, 64 * half + 64)
            fw.op("pe", lambda: nc.tensor.matmul(ps[0:kt_n, half * 256:(half + 1) * 256], KT[pr, g, kt_lo:kt_lo + kt_n],
                                                 QT[pr, 2 * g:2 * g + 2, j * 128:(j + 1) * 128], start=first, stop=True, skip_group_check=True),
                  [KT, QT], [ps])

    def exp_tile(self, ps, PT, rows, table=None, tmp=None):
        fw, nc = self.fw, self.nc
        if table is None:
            fw.op("act", lambda: nc.scalar.activation(PT[0:rows, :], ps[0:rows, :], AF.Exp, scale=0.125), [ps], [PT])
        else:
            tb, tap = table
            fw.op("dve", lambda: nc.vector.scalar_tensor_tensor(tmp[0:rows, :].rearrange("p (s q) -> p s q", s=4), ps[0:rows, :].rearrange("p (s q) -> p s q", s=4),
                                                                0.125, tap, ALU.mult, ALU.add), [ps, tb], [tmp])
            fw.op("act", lambda: nc.scalar.activation(PT[0:rows, :], tmp[0:rows, :], AF.Exp), [tmp], [PT])

    def phase_mix0(self):
        fw, nc = self.fw, self.nc
        d = self.dram
        FM, TM, O = d["FM0"], d["TM0"], d["O0"]
        tmv = TM.ap().rearrange("(t p) c -> p t c", p=128)
        with ExitStack() as es:
            tabs_t, cst_t = self.load_tabs(es)
            self.ebig_t = fw.sb(es, "ebig_t", [128, 4096], BF16)
            zeros = fw.sb(es, "zeros", [128, 512], BF16)
            fw.op("pool", lambda: nc.gpsimd.memset(zeros[:, :], 0.0), [], [zeros])
            W2 = fw.sb(es, "W2", [128, 32, 192], BF16)
            peT = fw.sb(es, "peT", [128, 32], BF16)
            KCT = fw.sb(es, "KCT", [128, 2, 256], BF16)
            with ExitStack() as es2:
                wst = fw.sb(es2, "cwst", [128, 32, 128], F32)
                pst = fw.sb(es2, "pst", [128, 32], F32)
                est = fw.sb(es2, "est", [128, 4096], F32)
                cw = d["cmp_w"].ap().rearrange("(l d) e -> d l e", d=64)
                for hf in range(2):
                    fw.dma("sp", wst[64 * hf:64 * hf + 64, :, :], cw, reads=[d["cmp_w"]], writes=[wst])
                    fw.dma("sp", pst[64 * hf:64 * hf + 64, :], d["cmp_pe"][:, :], reads=[d["cmp_pe"]], writes=[pst])
                fw.op("dve", lambda: nc.vector.tensor_copy(W2[:, :, 0:64], wst[:, :, 0:64]), [wst], [W2])
                fw.op("dve", lambda: nc.vector.tensor_copy(W2[:, :, 64:128], wst[:, :, 0:64]), [wst], [W2])
                fw.op("dve", lambda: nc.vector.tensor_copy(W2[:, :, 128:192], wst[:, :, 64:128]), [wst], [W2])
                fw.op("dve", lambda: nc.vector.tensor_copy(peT[:, :], pst[:, :]), [pst], [peT])
                fw.dma("sp", est[:, :], d["ebig"][:, :], reads=[d["ebig"]], writes=[est])
                fw.op("pool", lambda: nc.gpsimd.tensor_copy(self.ebig_t[:, :], est[:, :]), [est], [self.ebig_t])
                KCR = fw.sb(es2, "KCR", [128, 4096], BF16)
                VCR = fw.sb(es2, "VCR", [128, 4096], BF16)
                fw.dma("sp", KCR[:, :], FM[8 * 128:9 * 128, :], reads=[FM], writes=[KCR])
                fw.dma("sp", VCR[:, :], FM[9 * 128:10 * 128, :], reads=[FM], writes=[VCR])
                kps = fw.ps(es2, "kps", [128, 512], F32)
                kpe = fw.sb(es2, "kpe", [128, 2], F32)
                VCE = fw.sb(es2, "VCE", [128, 2, 2, 128], BF16)
                fw.op("pool", lambda: nc.gpsimd.memset(VCE[:, :, :, :], 0.0), [], [VCE])
                for g in range(2):
                    pr = slice(64 * g, 64 * g + 64)
                    for l in range(32):
                        fw.op("pe", lambda: nc.tensor.matmul(kps[:, 300:301], W2[pr, l, 0:128], peT[pr, l:l + 1], start=(l == 0), stop=(l == 31)), [W2, peT], [kps])
                    fw.op("dve", lambda: nc.vector.tensor_copy(kpe[:, g:g + 1], kps[:, 300:301]), [kps], [kpe])
                    for l in range(32):
                        fw.op("pe", lambda: nc.tensor.matmul(kps[:, 0:255], W2[pr, l, 0:128], KCR[pr, l:l + 16 * 254 + 1:16], start=(l == 0), stop=(l == 31)), [W2, KCR], [kps])
                    fw.op("dve", lambda: nc.vector.tensor_scalar(KCT[:, g, 0:255], kps[:, 0:255], kpe[:, g:g + 1], None, ALU.add), [kps, kpe], [KCT])
                    for ti in range(2):
                        nr = 128 if ti == 0 else 127
                        t0 = 16 * 128 * ti
                        for l in range(32):
                            fw.op("pe", lambda: nc.tensor.matmul(kps[0:nr, 384:448], VCR[pr, t0 + l:t0 + l + 16 * (nr - 1) + 1:16], W2[pr, l, 128:192], start=(l == 0), stop=False), [W2, VCR], [kps])
                        for l in range(32):
                            fw.op("pe", lambda: nc.tensor.matmul(kps[0:nr, 384:448], peT[pr, l:l + 1].to_broadcast([64, nr]), W2[pr, l, 128:192], start=False, stop=(l == 31)), [W2, peT], [kps])
                        fw.op("dve", lambda: nc.vector.tensor_copy(VCE[0:nr, ti, g, 0:64], kps[0:nr, 384:448]), [kps], [VCE])
                        fw.op("dve", lambda: nc.vector.tensor_copy(VCE[0:nr, ti, g, 64:128], cst_t[0:nr, C_OVW + ti * 64:C_OVW + (ti + 1) * 64]), [cst_t], [VCE])
                fw.dma("pool", d["VCD"].ap().rearrange("(t p) c -> p t c", p=128), VCE[:, :, :, :].rearrange("p t g c -> p t (g c)"), reads=[VCE], writes=[d["VCD"]])
                if "KCTD" in self.dbg:
                    fw.dma("pool", d["KCTD"][:, :], KCT[:, :, :].rearrange("p g n -> p (g n)"), reads=[KCT], writes=[d["KCTD"]])
            fw.barrier()
            VCF = fw.sb(es, "VCF", [128, 2, 256], BF16)
            VCN = fw.sb(es, "VCN", [16, NT, 256], BF16)
            fw.dma("sp", VCF[:, :, :], d["VCD"].ap().rearrange("(t p) c -> p t c", p=128), reads=[d["VCD"]], writes=[VCF])
            for j in range(NT):
                n0 = max(0, 8 * j - 9)
                nn = 8 * j + 7 - n0
                fw.dma("sp", VCN[0:nn, j, :], d["VCD"][n0:n0 + nn, :], reads=[d["VCD"]], writes=[VCN])
            QA = fw.sb(es, "QA", [128, 4, S], BF16)
            KS = fw.sb(es, "KS", [128, 2, S], BF16)
            KW = fw.sb(es, "KW", [128, 2, S], BF16)
            for c in range(4):
                fw.dma("sp", QA[:, c, :], FM[c * 128:(c + 1) * 128, :], reads=[FM], writes=[QA])
            for g in range(2):
                fw.dma("sp", KS[:, g, :], FM[(10 + g) * 128:(11 + g) * 128, :], reads=[FM], writes=[KS])
                fw.dma("sp", KW[:, g, :], FM[(12 + g) * 128:(13 + g) * 128, :], reads=[FM], writes=[KW])
            VS = fw.sb(es, "VS", [128, NT, 2, 65], BF16)
            VW = fw.sb(es, "VW", [128, NT, 2, 65], BF16)
            fw.op("pool", lambda: nc.gpsimd.memset(VS[:, :, :, :], 1.0), [], [VS])
            fw.op("pool", lambda: nc.gpsimd.memset(VW[:, :, :, :], 1.0), [], [VW])
            for g in range(2):
                fw.dma("sp", VS[:, :, g, 0:64], tmv[:, :, g * 64:(g + 1) * 64], reads=[TM], writes=[VS])
                fw.dma("sp", VW[:, :, g, 0:64], tmv[:, :, 128 + g * 64:128 + (g + 1) * 64], reads=[TM], writes=[VW])
            GS2 = fw.sb(es, "GS2", [128, NT, 2, 3, 4], F32)
            with ExitStack() as es2:
                GAb = fw.sb(es2, "GAb", [128, NT, 24], BF16)
                GAf = fw.sb(es2, "GAf", [128, NT, 24], F32)
                fw.dma("sp", GAb[:, :, :], tmv[:, :, 384:408], reads=[TM], writes=[GAb])
                fw.op("act", lambda: nc.scalar.activation(GAf[:, :, :], GAb[:, :, :], AF.Exp, scale=-1.0), [GAb], [GAf])
                fw.op("dve", lambda: nc.vector.tensor_scalar(GAf[:, :, :], GAf[:, :, :], 1.0, None, ALU.add), [GAf], [GAf])
                fw.op("dve", lambda: nc.vector.reciprocal(GAf[:, :, :], GAf[:, :, :]), [GAf], [GAf])
                for g in range(2):
                    for slot in range(4):
                        h = 4 * g + PERM[slot]
                        fw.op("dve", lambda: nc.vector.tensor_copy(GS2[:, :, g, :, slot], GAf[:, :, 3 * h:3 * h + 3]), [GAf], [GS2])
            fw.barrier()
            TA0, TA1 = self.build_bias_tables(es, tabs_t, cst_t, 0, False)
            TCs = []
            for nm, toff, moff in (("TCG", T_TCG, C_MCG), ("TC0", T_TC0, C_MC0), ("TC1", T_TC1, C_MC1)):
                tct = fw.sb(es, nm, [16, 8, 128], F32)
                for pos in range(8):
                    fw.op("dve", lambda: nc.vector.scalar_tensor_tensor(tct[0:16, pos, :], tabs_t[0:16, toff + pos * 128:toff + (pos + 1) * 128], tabs_t[0:16, T_B31 + pos:T_B31 + pos + 1],
                                                                        cst_t[0:16, moff:moff + 128], ALU.subtract, ALU.add), [tabs_t, cst_t], [tct])
                TCs.append(tct)
            MLT = fw.view(cst_t, "mlt")
            sc = [fw.ps(es, "sc%d" % i, [128, 512], F32) for i in range(3)]
            cps = fw.ps(es, "cps", [128, 512], F32)
            sps = fw.ps(es, "sps", [128, 512], F32)
            wps = fw.ps(es, "wps", [128, 512], F32)
            tps = fw.ps(es, "tps", [128, 1024], BF16)
            PTs = [fw.sb(es, "PT%d" % i, [128, 512], BF16) for i in range(6)]
            tmps = [fw.sb(es, "tmp%d" % i, [128, 512], F32) for i in range(3)]
            Fm = fw.sb(es, "Fm", [128, 64], F32)
            sm = {nm: [fw.sb(es, "%s%d" % (nm, i), shp, F32) for i in range(2)] for nm, shp in
                  (("den", [128, 4]), ("rden", [128, 4]), ("coef", [128, 4]), ("imp", [128, 64]), ("val", [128, 64]), ("m8", [128, 8]), ("thr", [128, 1]))}
            nmk = [fw.sb(es, "nmk%d" % i, [128, 64], BF16) for i in range(2)]
            NEGTs = [fw.sb(es, "NEGT%d" % i, [64, 4, 128], BF16) for i in range(2)]
            OAs = [fw.sb(es, "OA%d" % i, [128, 8, 64], F32) for i in range(2)]
            OBs = [fw.sb(es, "OAb%d" % i, [128, 512], BF16) for i in range(2)]
            cnt = {"sc": 0, "pt": 0, "tmp": 0, "u": 0}

            def nxt(key, lst):
                v = lst[cnt[key] % len(lst)]
                cnt[key] += 1
                return v

            def finish_branch(ps_acc, width, den_ap, br, j, g, OA, first):
                u = cnt["u"] % 2
                cnt["u"] += 1
                den, rden, coef = sm["den"][u], sm["rden"][u], sm["coef"][u]
                fw.op("dve", lambda: nc.vector.tensor_scalar(den[:, :], den_ap, 1e-30, None, ALU.max), [ps_acc], [den])
                fw.op("dve", lambda: nc.vector.reciprocal(rden[:, :], den[:, :]), [den], [rden])
                fw.op("dve", lambda: nc.vector.tensor_tensor(coef[:, :], rden[:, :], GS2[:, j, g, br, :], ALU.mult), [rden, GS2], [coef])
                for slot in range(4):
                    h = 4 * g + PERM[slot]
                    src = ps_acc[:, slot * width:slot * width + 64]
                    if first:
                        fw.op("dve", lambda: nc.vector.tensor_scalar(OA[:, h, :], src, coef[:, slot:slot + 1], None, ALU.mult), [ps_acc, coef], [OA])
                    else:
                        fw.op("dve", lambda: nc.vector.scalar_tensor_tensor(OA[:, h, :], src, coef[:, slot:slot + 1], OA[:, h, :], ALU.mult, ALU.add), [ps_acc, coef, OA], [OA])
                return rden

            for j in range(NT):
                OA = OAs[j % 2]
                fw.op("pool", lambda: nc.gpsimd.memset(Fm[:, :], 0.0), [], [Fm])
                fw.op("pool", lambda: nc.gpsimd.memset(Fm[:, 0:1], 1e4), [], [Fm])
                if j >= 1:
                    fw.op("pool", lambda: nc.gpsimd.memset(Fm[0:64, 2 * j - 1:2 * j + 1], 1e4), [], [Fm])
                fw.op("pool", lambda: nc.gpsimd.memset(Fm[0:64, 2 * j + 1:64], -1e30), [], [Fm])
                fw.op("pool", lambda: nc.gpsimd.memset(Fm[64:128, 2 * j:2 * j + 2], 1e4), [], [Fm])
                if 2 * j + 2 < 64:
                    fw.op("pool", lambda: nc.gpsimd.memset(Fm[64:128, 2 * j + 2:64], -1e30), [], [Fm])
                n0 = max(0, 8 * j - 9)
                nn = 8 * j + 7 - n0
                TC = TCs[1] if j == 0 else (TCs[2] if j == 1 else TCs[0])
                for g in range(2):
                    tiles = []
                    lo = 0
                    while lo < n0:
                        r = min(128, n0 - lo)
                        tiles.append((lo, r, "far"))
                        lo += r
                    tiles.append((n0, nn, "near"))
                    pts = []
                    for (lo, r, kind) in tiles:
                        ps = nxt("sc", sc)
                        self.score_tile(ps, KCT, g, lo, r, QA, j)
                        PT = nxt("pt", PTs)
                        if kind == "far":
                            self.exp_tile(ps, PT, r)
                        else:
                            self.exp_tile(ps, PT, r, (TC, TC[0:r, 4 * g:4 * g + 4, :]), nxt("tmp", tmps))
                        pts.append(PT)
                    for slot in range(4):
                        for i, (lo, r, kind) in enumerate(tiles):
                            rhs = VCF[0:r, lo // 128, g * 128:(g + 1) * 128] if kind == "far" else VCN[0:r, j, g * 128:(g + 1) * 128]
                            rb = VCF if kind == "far" else VCN
                            fw.op("pe", lambda: nc.tensor.matmul(cps[:, slot * 128:(slot + 1) * 128], pts[i][0:r, slot * 128:(slot + 1) * 128], rhs,
                                                                 start=(i == 0), stop=(i == len(tiles) - 1)), [pts[i], rb], [cps])
                    u = cnt["u"] % 2
                    den = sm["den"][u]
                    fw.op("dve", lambda: nc.vector.tensor_reduce(den[:, :], cps[:, :].rearrange("p (s c) -> p s c", s=4)[:, :, 64:128], AX.X, ALU.add), [cps], [den])
                    rden = finish_branch(cps, 128, den[:, :], 0, j, g, OA, True)
                    imp, val, m8, thr = sm["imp"][u], sm["val"][u], sm["m8"][u], sm["thr"][u]
                    fw.op("dve", lambda: nc.vector.tensor_scalar(imp[:, :], cps[:, 64:128], rden[:, 0:1], None, ALU.mult), [cps, rden], [imp])
                    for slot in range(1, 4):
                        fw.op("dve", lambda: nc.vector.scalar_tensor_tensor(imp[:, :], cps[:, slot * 128 + 64:slot * 128 + 128], rden[:, slot:slot + 1], imp[:, :], ALU.mult, ALU.add), [cps, rden, imp], [imp])
                    fw.op("dve", lambda: nc.vector.tensor_tensor(val[:, :], imp[:, :], Fm[:, :], ALU.add), [imp, Fm], [val])
                    fw.op("dve", lambda: nc.vector.max(out=m8[:, :], in_=val[:, :]), [val], [m8])
                    fw.op("dve", lambda: nc.vector.tensor_scalar(thr[:, :], m8[:, 7:8], -1e29, None, ALU.max), [m8], [thr])
                    fw.op("dve", lambda: nc.vector.tensor_scalar(val[:, :], val[:, :], thr[:, 0:1], None, ALU.is_lt), [val, thr], [val])
                    nk = nmk[u]
                    fw.op("dve", lambda: nc.vector.tensor_scalar(nk[:, :], val[:, :], 8.0 * NEG, None, ALU.mult), [val], [nk])
                    fw.op("pe", lambda: nc.tensor.transpose(tps[0:64, 0:128], nk[:, :], self.identb[:, :]), [nk, self.identb], [tps], kind="tr")
                    NEGT = NEGTs[u]
                    fw.op("dve", lambda: nc.vector.tensor_copy(NEGT[0:64, :, :], tps[0:64, 0:128].unsqueeze(1).to_broadcast([64, 4, 128])), [tps], [NEGT])
                    if "SELD" in self.dbg:
                        fw.dma("pool", d["SELD"][j * 128:(j + 1) * 128, g * 64:(g + 1) * 64], nk[:, :], reads=[nk], writes=[d["SELD"]])
                    fw.op("pe", lambda: nc.tensor.matmul(sps[:, 0:260], zeros[:, 0:128], zeros[:, 0:260], start=True, stop=False, skip_group_check=True), [zeros], [sps])
                    for kt in range(j + 1):
                        ps = nxt("sc", sc)
                        self.score_tile(ps, KS, g, kt * 128, 128, QA, j, mask=(self.ebig_t[0:64, kt * 128:(kt + 1) * 128], NEGT))
                        PT = nxt("pt", PTs)
                        if kt == j:
                            self.exp_tile(ps, PT, 128, (TA0, TA0[:, 4 * g:4 * g + 4, :]), nxt("tmp", tmps))
                        elif kt == j - 1:
                            self.exp_tile(ps, PT, 128, (TA1, TA1[:, 4 * g:4 * g + 4, :]), nxt("tmp", tmps))
                        else:
                            self.exp_tile(ps, PT, 128)
                        for slot in range(4):
                            fw.op("pe", lambda: nc.tensor.matmul(sps[:, slot * 65:(slot + 1) * 65], PT[:, slot * 128:(slot + 1) * 128], VS[:, kt, g, :],
                                                                 start=False, stop=(kt == j), skip_group_check=True), [PT, VS], [sps])
                    finish_branch(sps, 65, sps[:, 0:260].rearrange("p (s c) -> p s c", s=4)[:, :, 64], 1, j, g, OA, False)
                    fw.op("pe", lambda: nc.tensor.matmul(wps[:, 0:260], zeros[:, 0:128], zeros[:, 0:260], start=True, stop=False, skip_group_check=True), [zeros], [wps])
                    for kt in range(max(0, j - 4), j + 1):
                        ps = nxt("sc", sc)
                        self.score_tile(ps, KW, g, kt * 128, 128, QA, j)
                        PT = nxt("pt", PTs)
                        if kt == j:
                            self.exp_tile(ps, PT, 128, (TA0, TA0[:, 4 * g:4 * g + 4, :]), nxt("tmp", tmps))
                        elif kt == j - 1:
                            self.exp_tile(ps, PT, 128, (TA1, TA1[:, 4 * g:4 * g + 4, :]), nxt("tmp", tmps))
                        elif kt == j - 4:
                            self.exp_tile(ps, PT, 128, (MLT, cst_t[:, C_MASKLT:C_MASKLT + 128].unsqueeze(1).to_broadcast([128, 4, 128])), nxt("tmp", tmps))
                        else:
                            self.exp_tile(ps, PT, 128)
                        for slot in range(4):
                            fw.op("pe", lambda: nc.tensor.matmul(wps[:, slot * 65:(slot + 1) * 65], PT[:, slot * 128:(slot + 1) * 128], VW[:, kt, g, :],
                                                                 start=False, stop=(kt == j), skip_group_check=True), [PT, VW], [wps])
                    finish_branch(wps, 65, wps[:, 0:260].rearrange("p (s c) -> p s c", s=4)[:, :, 64], 2, j, g, OA, False)
                OB = OBs[j % 2]
                fw.op("pool", lambda: nc.gpsimd.tensor_copy(OB[:, :], OA[:, :, :].rearrange("p h c -> p (h c)")), [OA], [OB])
                fw.dma("pool", O[j * 128:(j + 1) * 128, 0:512], OB[:, :], reads=[OB], writes=[O])
        fw.barrier()
        with ExitStack() as es:
            tabs_t, cst_t = self.load_tabs(es)
            zeros = fw.sb(es, "zeros", [128, 512], BF16)
            fw.op("pool", lambda: nc.gpsimd.memset(zeros[:, :], 0.0), [], [zeros])
            QB = fw.sb(es, "QB", [128, 4, S], BF16)
            KB = fw.sb(es, "KB", [128, 2, S], BF16)
            for c in range(4):
                fw.dma("sp", QB[:, c, :], FM[(4 + c) * 128:(5 + c) * 128, :], reads=[FM], writes=[QB])
            for g in range(2):
                fw.dma("sp", KB[:, g, :], FM[(14 + g) * 128:(15 + g) * 128, :], reads=[FM], writes=[KB])
            VB = fw.sb(es, "VB", [128, NT, 2, 65], BF16)
            fw.op("pool", lambda: nc.gpsimd.memset(VB[:, :, :, :], 1.0), [], [VB])
            for g in range(2):
                fw.dma("sp", VB[:, :, g, 0:64], tmv[:, :, 256 + g * 64:256 + (g + 1) * 64], reads=[TM], writes=[VB])
            TB0, TB1 = self.build_bias_tables(es, tabs_t, cst_t, 8, True)
            snk = fw.sb(es, "snk", [128, 8], F32)
            snkE = fw.sb(es, "snkE", [128, 8], F32)
            self.load_bcast(snk, d["sinks"], d["sinks"][0:1, :])
            for g in range(2):
                for slot in range(4):
                    pos = 4 * g + slot
                    h = 4 * g + PERM[slot]
                    fw.op("dve", lambda: nc.vector.tensor_tensor(snkE[:, pos:pos + 1], snk[:, h:h + 1], tabs_t[:, T_B31 + 8 + pos:T_B31 + 8 + pos + 1], ALU.subtract), [snk, tabs_t], [snkE])
            fw.op("act", lambda: nc.scalar.activation(snkE[:, :], snkE[:, :], AF.Exp), [snkE], [snkE])
            sc = [fw.ps(es, "sc%d" % i, [128, 512], F32) for i in range(3)]
            bps = [fw.ps(es, "bps%d" % i, [128, 512], F32) for i in range(2)]
            PTs = [fw.sb(es, "PT%d" % i, [128, 512], BF16) for i in range(4)]
            tmps = [fw.sb(es, "tmp%d" % i, [128, 512], F32) for i in range(3)]
            dens = [fw.sb(es, "den%d" % i, [128, 4], F32) for i in range(2)]
            OAs = [fw.sb(es, "OB%d" % i, [128, 8, 64], BF16) for i in range(2)]
            c_sc = c_pt = c_tmp = c_u = 0
            for j in range(NT):
                OA = OAs[j % 2]
                for g in range(2):
                    bp = bps[c_u % 2]
                    den = dens[c_u % 2]
                    c_u += 1
                    fw.op("pe", lambda: nc.tensor.matmul(bp[:, 0:260], zeros[:, 0:128], zeros[:, 0:260], start=True, stop=False, skip_group_check=True), [zeros], [bp])
                    for kt in range(max(0, j - 1), j + 1):
                        ps = sc[c_sc % 3]; c_sc += 1
                        self.score_tile(ps, KB, g, kt * 128, 128, QB, j)
                        PT = PTs[c_pt % 4]; c_pt += 1
                        tm_ = tmps[c_tmp % 3]; c_tmp += 1
                        T = TB0 if kt == j else TB1
                        self.exp_tile(ps, PT, 128, (T, T[:, 4 * g:4 * g + 4, :]), tm_)
                        for slot in range(4):
                            fw.op("pe", lambda: nc.tensor.matmul(bp[:, slot * 65:(slot + 1) * 65], PT[:, slot * 128:(slot + 1) * 128], VB[:, kt, g, :],
                                                                 start=False, stop=(kt == j), skip_group_check=True), [PT, VB], [bp])
                    fw.op("dve", lambda: nc.vector.tensor_tensor(den[:, :], bp[:, 0:260].rearrange("p (s c) -> p s c", s=4)[:, :, 64], snkE[:, 4 * g:4 * g + 4], ALU.add), [bp, snkE], [den])
                    fw.op("dve", lambda: nc.vector.reciprocal(den[:, :], den[:, :]), [den], [den])
                    for slot in range(4):
                        h = 4 * g + PERM[slot]
                        fw.op("dve", lambda: nc.vector.tensor_scalar(OA[:, h, :], bp[:, slot * 65:slot * 65 + 64], den[:, slot:slot + 1], None, ALU.mult), [bp, den], [OA])
                fw.dma("pool", O[j * 128:(j + 1) * 128, 512:1024], OA[:, :, :].rearrange("p h c -> p (h c)"), reads=[OA], writes=[O])
        fw.barrier()

    def phase_mix1(self):
        fw, nc = self.fw, self.nc
        d = self.dram
        FM, FMF, TM, O = d["FM1"], d["FMF"], d["TM1"], d["O1"]
        tmv = TM.ap().rearrange("(t p) c -> p t c", p=128)
        with ExitStack() as es:
            tabs_t, cst_t = self.load_tabs(es)
            TA0, TA1 = self.build_bias_tables(es, tabs_t, cst_t, 0, False)
            KMT = fw.sb(es, "KMT", [128, 2, 16], F32)
            with ExitStack() as es2:
                kf = fw.sb(es2, "kf", [128, 4096], F32)
                for g in range(2):
                    fw.dma("sp", kf[:, :], FMF[(4 + g) * 128:(5 + g) * 128, :], reads=[FMF], writes=[kf])
                    fw.op("dve", lambda: nc.vector.tensor_reduce(KMT[:, g, :], kf[:, :].rearrange("p (n l) -> p n l", l=256), AX.X, ALU.add), [kf], [KMT])
                fw.op("dve", lambda: nc.vector.tensor_scalar(KMT[:, :, :], KMT[:, :, :], 1.0 / 256.0, None, ALU.mult), [KMT], [KMT])
            fw.barrier()
            QC = fw.sb(es, "QC", [128, 4, S], BF16)
            KC = fw.sb(es, "KC", [128, 2, S], BF16)
            for c in range(4):
                fw.dma("sp", QC[:, c, :], FM[c * 128:(c + 1) * 128, :], reads=[FM], writes=[QC])
            for g in range(2):
                fw.dma("sp", KC[:, g, :], FM[(4 + g) * 128:(5 + g) * 128, :], reads=[FM], writes=[KC])
            VC = fw.sb(es, "VC", [128, NT, 2, 65], BF16)
            fw.op("pool", lambda: nc.gpsimd.memset(VC[:, :, :, :], 1.0), [], [VC])
            for g in range(2):
                fw.dma("sp", VC[:, :, g, 0:64], tmv[:, :, g * 64:(g + 1) * 64], reads=[TM], writes=[VC])
            sc = [fw.ps(es, "sc%d" % i, [128, 512], F32) for i in range(3)]
            bps = [fw.ps(es, "bps%d" % i, [128, 512], F32) for i in range(2)]
            gps = fw.ps(es, "gps", [128, 512], F32)
            PTs = [fw.sb(es, "PT%d" % i, [128, 512], BF16) for i in range(6)]
            tmps = [fw.sb(es, "tmp%d" % i, [128, 512], F32) for i in range(3)]
            qfs = [fw.sb(es, "qf%d" % i, [128, 4, 128], F32) for i in range(2)]
            OM = fw.sb(es, "OM", [128, 8, 16], F32)
            LT = fw.sb(es, "LT", [128, 8, 16], F32)
            gsm = [fw.sb(es, "gsm%d" % i, [128, 8, 16], F32) for i in range(2)]
            sel = [fw.sb(es, "sel%d" % i, [128, 8, 16], F32) for i in range(2)]
            m8 = [fw.sb(es, "m8%d" % i, [128, 8, 8], F32) for i in range(2)]
            OAs = [fw.sb(es, "OAc%d" % i, [128, 8, 65], F32) for i in range(2)]
            rdn = [fw.sb(es, "rdn%d" % i, [128, 8], F32) for i in range(2)]
            OBs = [fw.sb(es, "OCb%d" % i, [128, 8, 64], BF16) for i in range(2)]
            c_sc = c_pt = c_tmp = c_b = 0
            fmfv = FMF.ap()[0:512, :].rearrange("(c p) q -> p c q", p=128)
            for j in range(NT):
                own = j // 2
                u = j % 2
                if j % 2 == 0:
                    fw.op("pool", lambda: nc.gpsimd.memset(OM[:, :, :], -1e30), [], [OM])
                    fw.op("pool", lambda: nc.gpsimd.memset(LT[:, :, :], 0.0), [], [LT])
                    if own > 0:
                        fw.op("pool", lambda: nc.gpsimd.memset(OM[:, :, 0:own], 0.0), [], [OM])
                        fw.op("pool", lambda: nc.gpsimd.memset(LT[:, :, 0:own], 1.0), [], [LT])
                qf = qfs[u]
                fw.dma("sp", qf[:, :, :], fmfv[:, :, j * 128:(j + 1) * 128], reads=[FMF], writes=[qf])
                for h in range(8):
                    pr = slice(64 * (h % 2), 64 * (h % 2) + 64)
                    fw.op("pe", lambda: nc.tensor.matmul(gps[:, h * 16:(h + 1) * 16], qf[pr, h // 2, :], KMT[pr, h // 4, :], start=True, stop=True), [qf, KMT], [gps], kind="f32")
                G_, S_, M_ = gsm[u], sel[u], m8[u]
                fw.op("dve", lambda: nc.vector.tensor_tensor(G_[:, :, :], gps[:, 0:128].rearrange("p (h n) -> p h n", h=8), OM[:, :, :], ALU.add), [gps, OM], [G_])
                for h in range(8):
                    fw.op("dve", lambda: nc.vector.max(out=M_[:, h, :], in_=G_[:, h, :]), [G_], [M_])
                fw.op("dve", lambda: nc.vector.tensor_tensor(S_[:, :, :], G_[:, :, :], M_[:, :, 2:3].to_broadcast([128, 8, 16]), ALU.is_ge), [G_, M_], [S_])
                fw.op("dve", lambda: nc.vector.tensor_tensor(S_[:, :, :], S_[:, :, :], LT[:, :, :], ALU.mult), [S_, LT], [S_])
                fw.op("dve", lambda: nc.vector.memset(S_[:, :, own:own + 1], 1.0), [], [S_])
                if "GSM1" in self.dbg:
                    if not dict.__contains__(d, "GSM1"):
                        self.dscr("GSM1", [S, 128], F32)
                    fw.dma("pool", d["GSM1"][j * 128:(j + 1) * 128, :], G_[:, :, :].rearrange("p h n -> p (h n)"), reads=[G_], writes=[d["GSM1"]])
                if "SEL1" in self.dbg:
                    fw.dma("pool", d["SEL1"][j * 128:(j + 1) * 128, :], S_[:, :, :].rearrange("p h n -> p (h n)"), reads=[S_], writes=[d["SEL1"]])
                OA = OAs[u]
                for g in range(2):
                    for n in range(own + 1):
                        kts = [kt for kt in (2 * n, 2 * n + 1) if kt <= j]
                        pts = []
                        for kt in kts:
                            ps = sc[c_sc % 3]; c_sc += 1
                            self.score_tile(ps, KC, g, kt * 128, 128, QC, j)
                            PT = PTs[c_pt % 6]; c_pt += 1
                            if kt == j:
                                tm_ = tmps[c_tmp % 3]; c_tmp += 1
                                self.exp_tile(ps, PT, 128, (TA0, TA0[:, 4 * g:4 * g + 4, :]), tm_)
                            elif kt == j - 1:
                                tm_ = tmps[c_tmp % 3]; c_tmp += 1
                                self.exp_tile(ps, PT, 128, (TA1, TA1[:, 4 * g:4 * g + 4, :]), tm_)
                            else:
                                self.exp_tile(ps, PT, 128)
                            pts.append(PT)
                        bp = bps[c_b % 2]; c_b += 1
                        for slot in range(4):
                            for i, kt in enumerate(kts):
                                fw.op("pe", lambda: nc.tensor.matmul(bp[:, slot * 65:(slot + 1) * 65], pts[i][:, slot * 128:(slot + 1) * 128], VC[:, kt, g, :],
                                                                     start=(i == 0), stop=(i == len(kts) - 1)), [pts[i], VC], [bp])
                        for slot in range(4):
                            h = 4 * g + PERM[slot]
                            if n == 0:
                                fw.op("dve", lambda: nc.vector.tensor_scalar(OA[:, h, :], bp[:, slot * 65:(slot + 1) * 65], S_[:, h, n:n + 1], None, ALU.mult), [bp, S_], [OA])
                            else:
                                fw.op("dve", lambda: nc.vector.scalar_tensor_tensor(OA[:, h, :], bp[:, slot * 65:(slot + 1) * 65], S_[:, h, n:n + 1], OA[:, h, :], ALU.mult, ALU.add), [bp, S_, OA], [OA])
                rd = rdn[u]
                OB = OBs[u]
                fw.op("dve", lambda: nc.vector.reciprocal(rd[:, :], OA[:, :, 64]), [OA], [rd])
                fw.op("pool", lambda: nc.gpsimd.tensor_tensor(OB[:, :, :], OA[:, :, 0:64], rd[:, :].unsqueeze(2).to_broadcast([128, 8, 64]), ALU.mult), [OA, rd], [OB])
                fw.dma("pool", O[j * 128:(j + 1) * 128, 0:512], OB[:, :, :].rearrange("p h c -> p (h c)"), reads=[OB], writes=[O])
        fw.barrier()
        with ExitStack() as es:
            NTRI = fw.sb(es, "NTRI", [128, 128], BF16)
            MS = fw.sb(es, "MS", [128, 128], BF16)
            ones = fw.sb(es, "ones", [128, 1], BF16)
            with ExitStack() as es2:
                cst_t = fw.sb(es2, "cst_t", [128, C_END], F32)
                fw.dma("sp", cst_t[:, :], d["consts"][:, :], reads=[d["consts"]], writes=[cst_t])
                fw.op("dve", lambda: nc.vector.tensor_scalar(NTRI[:, :], cst_t[:, C_TRI:C_TRI + 128], 8.0, None, ALU.mult), [cst_t], [NTRI])
                fw.op("dve", lambda: nc.vector.tensor_copy(MS[:, :], cst_t[:, C_SBM:C_SBM + 128]), [cst_t], [MS])
            fw.barrier()
            fw.op("pool", lambda: nc.gpsimd.memset(ones[:, :], 1.0), [], [ones])
            QD = fw.sb(es, "QD", [128, 4, S], BF16)
            KD = fw.sb(es, "KD", [128, 4, S], BF16)
            VD = fw.sb(es, "VD", [128, NT, 512], BF16)
            for c in range(4):
                fw.dma("sp", QD[:, c, :], FM[(6 + c) * 128:(7 + c) * 128, :], reads=[FM], writes=[QD])
                fw.dma("sp", KD[:, c, :], FM[(10 + c) * 128:(11 + c) * 128, :], reads=[FM], writes=[KD])
            fw.dma("sp", VD[:, :, :], tmv[:, :, 128:640], reads=[TM], writes=[VD])
            zps = [fw.ps(es, "zps%d" % i, [128, 512], F32) for i in range(3)]
            ops_ = [fw.ps(es, "ops%d" % i, [128, 512], F32) for i in range(2)]
            E32 = [fw.sb(es, "E32_%d" % i, [128, 512], F32) for i in range(2)]
            SPM = [fw.sb(es, "SPM%d" % i, [128, 512], BF16) for i in range(3)]
            WT = [fw.sb(es, "WT%d" % i, [128, 512], BF16) for i in range(3)]
            Rs = [fw.sb(es, "R%d" % i, [128, 4], F32) for i in range(2)]
            Fs = [fw.sb(es, "F%d" % i, [128, 4], F32) for i in range(3)]
            OAs = [fw.sb(es, "OD%d" % i, [128, 4, 64], F32) for i in range(2)]
            OGs = [fw.sb(es, "OG%d" % i, [128, 4, 512], BF16) for i in range(2)]
            cz = co = cf = ch = 0
            for G in range(8):
                OG = OGs[G % 2]
                for h in range(8):
                    pr = slice(64 * (h % 2), 64 * (h % 2) + 64)
                    pair = h // 2
                    R = Rs[ch % 2]
                    OA = OAs[ch % 2]
                    ch += 1
                    fw.op("pool", lambda: nc.gpsimd.memset(R[:, :], 0.0), [], [R])
                    for kt in range(4 * G + 3, -1, -1):
                        i0 = max(0, kt - 4 * G)
                        c0 = i0 * 128
                        diag = kt >= 4 * G
                        zp = zps[cz % 3]
                        e32 = E32[cz % 2]
                        spm = SPM[cz % 3]
                        wt = WT[cz % 3]
                        cz += 1
                        fw.op("pe", lambda: nc.tensor.matmul(zp[:, c0:512], KD[pr, pair, kt * 128:(kt + 1) * 128], QD[pr, pair, G * 512 + c0:(G + 1) * 512],
                                                             start=True, stop=False, skip_group_check=True), [KD, QD], [zp])
                        fw.op("act", lambda: nc.scalar.activation(e32[:, c0:512], zp[:, c0:512], AF.Exp, scale=0.125), [zp], [e32])
                        fw.op("act", lambda: nc.scalar.activation(spm[:, c0:512], e32[:, c0:512], AF.Ln, bias=1.0), [e32], [spm])
                        if diag:
                            fw.op("pool", lambda: nc.gpsimd.tensor_tensor(spm[:, c0:c0 + 128], spm[:, c0:c0 + 128], MS[:, :], ALU.mult), [spm, MS], [spm])
                        fw.op("pe", lambda: nc.tensor.matmul(zp[:, c0:512], NTRI[:, :], spm[:, c0:512], start=False, stop=True, skip_group_check=True), [NTRI, spm], [zp])
                        fw.op("act", lambda: nc.scalar.activation(wt[:, c0:512], zp[:, c0:512], AF.Exp, scale=0.125), [zp], [wt])
                        if diag:
                            fw.op("pool", lambda: nc.gpsimd.tensor_tensor(wt[:, c0:c0 + 128], wt[:, c0:c0 + 128], MS[:, :], ALU.mult), [wt, MS], [wt])
                        op_ = ops_[co % 2]
                        co += 1
                        for i in range(i0, 4):
                            fw.op("pe", lambda: nc.tensor.matmul(op_[:, i * 65:i * 65 + 64], wt[:, i * 128:(i + 1) * 128], VD[:, kt, h * 64:(h + 1) * 64], start=True, stop=True), [wt, VD], [op_])
                            fw.op("pe", lambda: nc.tensor.matmul(op_[:, i * 65 + 64:i * 65 + 65], spm[:, i * 128:(i + 1) * 128], ones[:, 0:1], start=True, stop=True), [spm, ones], [op_])
                        F = Fs[cf % 3]
                        cf += 1
                        fw.op("act", lambda: nc.scalar.activation(F[:, :], R[:, :], AF.Exp, scale=-1.0), [R], [F])
                        if G == 0 and h == 0 and kt in (3, 0):
                            self.dump("D_E32_%d" % kt, e32, e32[:, :], [128, 512])
                            self.dump("D_SPM_%d" % kt, spm, spm[:, :], [128, 512], BF16)
                            self.dump("D_WT_%d" % kt, wt, wt[:, :], [128, 512], BF16)
                            self.dump("D_F_%d" % kt, F, F[:, :], [128, 4])
                            if ("D_OP_%d" % kt) in self.dbg:
                                dtmp = fw.sb(es, "dtmp%d" % kt, [128, 512], F32)
                                fw.op("dve", lambda: nc.vector.tensor_copy(dtmp[:, 0:260], op_[:, 0:260]), [op_], [dtmp])
                                self.dump("D_OP_%d" % kt, dtmp, dtmp[:, :], [128, 512])
                                dtmp2 = fw.sb(es, "dtmpz%d" % kt, [128, 512], F32)
                                fw.op("dve", lambda: nc.vector.tensor_copy(dtmp2[:, :], zp[:, :]), [zp], [dtmp2])
                                self.dump("D_ZP_%d" % kt, dtmp2, dtmp2[:, :], [128, 512])
                        for i in range(i0, 4):
                            if diag and i == i0:
                                fw.op("dve", lambda: nc.vector.tensor_copy(OA[:, i, :], op_[:, i * 65:i * 65 + 64]), [op_], [OA])
                            else:
                                fw.op("dve", lambda: nc.vector.scalar_tensor_tensor(OA[:, i, :], op_[:, i * 65:i * 65 + 64], F[:, i:i + 1], OA[:, i, :], ALU.mult, ALU.add), [op_, F, OA], [OA])
                        fw.op("dve", lambda: nc.vector.tensor_tensor(R[:, i0:4], R[:, i0:4], op_[:, 0:260].rearrange("p (s c) -> p s c", s=4)[:, i0:4, 64], ALU.add), [R, op_], [R])
                    if G == 0 and h == 0:
                        self.dump("D_OA", OA, OA[:, :, :].rearrange("p i c -> p (i c)"), [128, 256])
                        self.dump("D_R", R, R[:, :], [128, 4])
                    fw.op("pool", lambda: nc.gpsimd.tensor_copy(OG[:, :, h * 64:(h + 1) * 64], OA[:, :, :]), [OA], [OG])
                fw.dma("pool", O.ap()[G * 512:(G + 1) * 512, 512:1024].rearrange("(i p) c -> p i c", p=128), OG[:, :, :], reads=[OG], writes=[O])
        fw.barrier()

    def declare(self):
        specs = {}
        def I(name, shape, dt=F32):
            specs[name] = ("in", shape, dt)
        def Sc(name, shape, dt=BF16, out=False):
            specs[name] = ("out" if out else "scr", shape, dt)
        I("x", [S, D]); I("cT", [128, 8]); I("tabs", [128, T_END]); I("consts", [128, C_END]); I("ebig", [128, 4096])
        I("mod_w", [4 * 1024, 3072]); I("mod_b", [4, 3072]); I("norm_w", [4, 2048])
        I("w_in_ab", [1024, 2072]); I("w_out_ab", [1024, 1024]); I("cmp_w", [32 * 64, 128]); I("cmp_pe", [64, 32]); I("sinks", [1, 8])
        I("w_in_cd", [1024, 2304]); I("w_out_cd", [1024, 1024]); I("ffn_w_in", [2 * 1024, 2 * DFF]); I("ffn_w_out", [2 * DFF, 1024])
        Sc("MODV", [4, 3072], F32); Sc("FM0", [16 * 128, S]); Sc("TM0", [S, 408]); Sc("FM1", [14 * 128, S]); Sc("FMF", [6 * 128, S], F32)
        Sc("TM1", [S, 640]); Sc("VCD", [256, 256]); Sc("KCTD", [128, 512]); Sc("SELD", [S, 128]); Sc("SEL1", [S, 128], F32)
        Sc("O0", [S, D]); Sc("O1", [S, D]); Sc("X1", [S, D], F32); Sc("X2", [S, D], F32); Sc("X3", [S, D], F32); Sc("Y", [S, D], F32, out=True)
        prog = self

        class Lazy(dict):
            def __missing__(self, name):
                kind, shape, dt = specs[name]
                if kind == "in":
                    prog.din(name, shape, dt)
                else:
                    prog.dscr(name, shape, dt, out=(kind == "out"))
                return dict.__getitem__(self, name)

        self.dram = Lazy()
        self.in_names = []
        self.out_names = []


def build(phases, dbg=(), xin_override=None, as_input=()):
    p = Prog(dbg, as_input)
    p.declare()
    d = p.dram
    nc = p.nc
    with ExitStack() as es:
        p.fw = FW(nc, es)
        p.phase_setup(es)
        for ph in phases:
            p.fw.pe_pipeline = ph in ("proj0", "proj1", "out0", "out1", "ffn0", "ffn1")
            if ph == "modvec":
                p.phase_modvec()
            elif ph == "proj0":
                p.phase_proj(0, d["x"])
            elif ph == "proj1":
                p.phase_proj(1, d["X2"] if xin_override is None else d[xin_override])
            elif ph == "mix0":
                p.phase_mix0()
            elif ph == "mix1":
                p.phase_mix1()
            elif ph == "out0":
                p.phase_outproj(0, d["x"], d["X1"])
            elif ph == "out1":
                p.phase_outproj(1, d["X2"] if xin_override is None else d[xin_override], d["X3"])
            elif ph == "ffn0":
                p.phase_ffn(0, d["X1"] if xin_override is None else d[xin_override], d["X2"])
            elif ph == "ffn1":
                p.phase_ffn(1, d["X3"] if xin_override is None else d[xin_override], d["Y"])
            else:
                raise ValueError(ph)
        p.fw.barrier()
    return p


ALL_PHASES = ["modvec", "proj0", "mix0", "out0", "ffn0", "proj1", "mix1", "out1", "ffn1"]


def host_inputs(inputs):
    f = lambda a: np.ascontiguousarray(np.asarray(a, dtype=np.float32))
    rel = f(inputs["rel_table"])
    tabs, consts, ebig = _host_tables(rel)
    shared = {
        "tabs": tabs, "consts": consts, "ebig": ebig,
        "mod_w": f(inputs["mod_w"]).reshape(4 * 1024, 3072),
        "mod_b": f(inputs["mod_b"]).reshape(4, 3072),
        "norm_w": f(inputs["norm_w"]).reshape(4, 2048),
        "w_in_ab": f(inputs["w_in_ab"])[0],
        "w_out_ab": f(inputs["w_out_ab"])[0],
        "cmp_w": np.ascontiguousarray(np.concatenate([f(inputs["nsa_cmp_wk"])[0].reshape(2048, 64),
                                                      f(inputs["nsa_cmp_wv"])[0].reshape(2048, 64)], axis=1)),
        "cmp_pe": np.ascontiguousarray(f(inputs["nsa_cmp_pe"])[0].T),
        "sinks": f(inputs["swa_sinks"]).reshape(1, 8),
        "w_in_cd": f(inputs["w_in_cd"])[0],
        "w_out_cd": f(inputs["w_out_cd"])[0],
        "ffn_w_in": f(inputs["ffn_w_in"]).reshape(2 * 1024, 2 * DFF),
        "ffn_w_out": f(inputs["ffn_w_out"]).reshape(2 * DFF, 1024),
    }
    x = f(inputs["x"])
    c = f(inputs["c"])
    maps = []
    for b in range(x.shape[0]):
        m = dict(shared)
        m["x"] = x[b]
        m["cT"] = np.ascontiguousarray(c[b].reshape(8, 128).T)
        maps.append(m)
    return maps


LAUNCHES = [
    (list(ALL_PHASES), set(), set()),
]


def kernel(**inputs):
    maps = host_inputs(inputs)
    n = len(maps)
    carry = [dict() for _ in range(n)]
    res = None
    for phases, outs, as_in in LAUNCHES:
        p = build(phases, dbg=outs, as_input=as_in)
        in_maps = []
        for c in range(n):
            m = {}
            for nm in p.in_names:
                m[nm] = carry[c][nm] if nm in carry[c] else maps[c][nm]
            in_maps.append(m)
        res = run_bass_kernel_spmd(p.nc, in_maps, core_ids=list(range(n)))
        for c in range(n):
            for nm in p.out_names:
                carry[c][nm] = np.ascontiguousarray(np.asarray(res.results[c][nm]))
    return np.stack([np.asarray(carry[c]["Y"], dtype=np.float32) for c in range(n)], axis=0)
```
